# Optimizing a Trainium2 kernel written in Bass

```python
import math
import jax, jax.numpy as jnp
from jax import lax
import numpy as np

D_MODEL = 1024
BATCH = 2
SEQ = 8192
DEPTH = 4

CHUNK = 64
Q_BLOCK = 128
ROPE_THETA = 10000.0
EPS = 1e-6
D_FF = 2816
N_BRANCH = 4
MIX_WIDTH = 512

DA_HEADS = 4
DA_QK_DIM = 64
DA_V_DIM = 128
GLA_HEADS = 4
GLA_DK = 64
GLA_DV = 128
GLA_GATE_RANK = 16
GLA_GATE_TAU = 16.0
SSD_HEADS = 8
SSD_HEAD_DIM = 64
SSD_GROUPS = 2
SSD_STATE = 128
SSD_CONV = 4
SSD_INNER = SSD_HEADS * SSD_HEAD_DIM
SSD_CONV_DIM = SSD_INNER + 2 * SSD_GROUPS * SSD_STATE
RW_HEADS = 8
RW_HEAD = 64
RW_DIM = RW_HEADS * RW_HEAD
RW_DECAY_RANK = 64
RW_A_RANK = 64
RW_GATE_RANK = 128
RW_DECAY_SCALE = 0.606531
RW_GN_EPS = 64e-5

DA_SIZES = (DA_HEADS * 2 * DA_QK_DIM, DA_HEADS * 2 * DA_QK_DIM, DA_HEADS * DA_V_DIM)
GLA_SIZES = (GLA_HEADS * GLA_DK, GLA_HEADS * GLA_DK, GLA_HEADS * GLA_DV, GLA_HEADS * GLA_DV, GLA_GATE_RANK)
SSD_SIZES = (SSD_INNER, SSD_CONV_DIM, SSD_HEADS)
RW_SIZES = (RW_DIM, RW_DIM, RW_DIM, RW_DECAY_RANK, RW_A_RANK, RW_GATE_RANK)
GATE_COLS = N_BRANCH * D_MODEL
MIXER_COLS = (sum(DA_SIZES), sum(GLA_SIZES), sum(SSD_SIZES), sum(RW_SIZES), GATE_COLS)
N_IN = sum(MIXER_COLS)

kernel_name = "hybrid_gated_diffattn_gla_ssd_rwkv7_macaron"


def rmsnorm(x, g, eps=EPS):
    xf = x.astype(jnp.float32)
    y = xf * lax.rsqrt(jnp.mean(jnp.square(xf), axis=-1, keepdims=True) + eps)
    return (y * g.astype(jnp.float32)).astype(x.dtype)


def swiglu_ffn(x, wg, wu, wd):
    return (jax.nn.silu(x @ wg) * (x @ wu)) @ wd


def split_cols(t, sizes):
    return jnp.split(t, np.cumsum(sizes)[:-1].tolist(), axis=-1)


def token_shift(t):
    return jnp.pad(t, ((0, 0), (1, 0), (0, 0)))[:, :-1]


def to_chunks(t):
    B, S, H = t.shape[:3]
    t = t.reshape((B, S // CHUNK, CHUNK, H) + t.shape[3:])
    return jnp.moveaxis(t, (1, 3), (0, 2))


def from_chunks(t):
    n, B, H, C, d = t.shape
    return jnp.moveaxis(t, (0, 2), (1, 3)).reshape(B, n * C, H, d)


def rope(t):
    S, d = t.shape[1], t.shape[-1]
    half = d // 2
    inv_freq = ROPE_THETA ** (-jnp.arange(half, dtype=jnp.float32) / half)
    ang = jnp.arange(S, dtype=jnp.float32)[:, None] * inv_freq[None, :]
    cos = jnp.cos(ang)[None, :, None, :]
    sin = jnp.sin(ang)[None, :, None, :]
    tf = t.astype(jnp.float32)
    t1, t2 = tf[..., :half], tf[..., half:]
    return jnp.concatenate([t1 * cos - t2 * sin, t2 * cos + t1 * sin], axis=-1).astype(t.dtype)


def causal_depthwise_conv(x, w, b):
    K, C = w.shape
    xp = jnp.pad(x, ((0, 0), (K - 1, 0), (0, 0)))
    y = lax.conv_general_dilated(xp, w[:, None, :].astype(x.dtype), window_strides=(1,), padding='VALID',
                                 dimension_numbers=('NWC', 'WIO', 'NWC'), feature_group_count=C)
    return y + b


def differential_attention(q, k, v, lam, lam_init, norm_g):
    B, S, H, _, dq = q.shape
    q = rope(q.reshape(B, S, H * 2, dq)).reshape(B, S, H, 2, dq) * (dq ** -0.5)
    k = rope(k.reshape(B, S, H * 2, dq)).reshape(B, S, H, 2, dq)
    n_blk = S // Q_BLOCK
    q_blocks = jnp.moveaxis(q.reshape(B, n_blk, Q_BLOCK, H, 2, dq), 1, 0)
    key_chunk = jnp.arange(S) // CHUNK

    def attend(args):
        q_blk, blk = args
        query_chunk = (blk * Q_BLOCK + jnp.arange(Q_BLOCK)) // CHUNK
        allowed = key_chunk[None, :] <= query_chunk[:, None]
        s = jnp.einsum('bqhcd,bkhcd->bhcqk', q_blk, k).astype(jnp.float32)
        p = jax.nn.softmax(jnp.where(allowed, s, -jnp.inf), axis=-1)
        a = p[:, :, 0] - lam * p[:, :, 1]
        return jnp.einsum('bhqk,bkhd->bqhd', a.astype(v.dtype), v)

    o = lax.map(attend, (q_blocks, jnp.arange(n_blk)))
    o = jnp.moveaxis(o, 0, 1).reshape(B, S, H, -1)
    o = rmsnorm(o, norm_g) * (1.0 - lam_init)
    return o.reshape(B, S, -1)


def gla_chunked(q, k, v, log_g):
    B, S, H, dk = q.shape
    dv = v.shape[-1]
    f32 = jnp.float32
    qc, kc, vc, gc = (to_chunks(t.astype(f32)) for t in (q, k, v, log_g))
    G = jnp.cumsum(gc, axis=3)
    G_ref = G[:, :, :, CHUNK // 2:CHUNK // 2 + 1]
    causal = jnp.tril(jnp.ones((CHUNK, CHUNK), bool))
    att = jnp.einsum('cbhik,cbhjk->cbhij', qc * jnp.exp(G - G_ref), kc * jnp.exp(G_ref - G))
    att = jnp.where(causal, att, 0.0)
    y_intra = jnp.einsum('cbhij,cbhjv->cbhiv', att, vc)
    G_last = G[:, :, :, -1]
    chunk_kv = jnp.einsum('cbhjk,cbhjv->cbhkv', kc * jnp.exp(G_last[:, :, :, None] - G), vc)
    q_in = qc * jnp.exp(G)

    def step(state, inp):
        dec, kv, qi = inp
        y = jnp.einsum('bhik,bhkv->bhiv', qi, state)
        return jnp.exp(dec)[..., None] * state + kv, y

    _, y_inter = lax.scan(step, jnp.zeros((B, H, dk, dv), f32), (G_last, chunk_kv, q_in))
    return from_chunks(y_intra + y_inter)


def ssd_chunked(x, dt, A, bm, cm):
    B, S, H, P = x.shape
    N = bm.shape[-1]
    f32 = jnp.float32
    xc = to_chunks(x.astype(f32) * dt[..., None])
    bc = to_chunks(bm.astype(f32))
    cc = to_chunks(cm.astype(f32))
    acum = jnp.cumsum(to_chunks(dt * A), axis=-1)
    causal = jnp.tril(jnp.ones((CHUNK, CHUNK), bool))
    L = jnp.exp(jnp.where(causal, acum[..., :, None] - acum[..., None, :], -jnp.inf))
    scores = jnp.einsum('cbhin,cbhjn->cbhij', cc, bc) * L
    y_intra = jnp.einsum('cbhij,cbhjp->cbhip', scores, xc)
    decay_to_end = jnp.exp(acum[..., -1:] - acum)
    chunk_states = jnp.einsum('cbhjn,cbhj,cbhjp->cbhpn', bc, decay_to_end, xc)
    c_in = cc * jnp.exp(acum)[..., None]
    chunk_decay = jnp.exp(acum[..., -1])

    def step(h, inp):
        dec, st, ci = inp
        y = jnp.einsum('bhin,bhpn->bhip', ci, h)
        return dec[..., None, None] * h + st, y

    _, y_inter = lax.scan(step, jnp.zeros((B, H, P, N), f32), (chunk_decay, chunk_states, c_in))
    return from_chunks(y_intra + y_inter)


def rwkv7_recurrence(r, w, k, v, kk, a):
    B, S, H, N = r.shape

    def step(state, inp):
        r_t, w_t, k_t, v_t, kk_t, a_t = inp
        sa = jnp.einsum('bhvk,bhk->bhv', state, -kk_t)
        state = (state * w_t[:, :, None, :] + sa[..., None] * (kk_t * a_t)[:, :, None, :]
                 + v_t[..., None] * k_t[:, :, None, :])
        return state, jnp.einsum('bhvk,bhk->bhv', state, r_t)

    xs = tuple(jnp.moveaxis(t.astype(jnp.float32), 1, 0) for t in (r, w, k, v, kk, a))
    _, y = lax.scan(step, jnp.zeros((B, H, N, N), jnp.float32), xs)
    return jnp.moveaxis(y, 0, 1)


def hybrid_token_mixer(u, lidx, w_in, da_lambda_q1, da_lambda_k1, da_lambda_q2, da_lambda_k2, da_norm,
                       gla_gate_w2, gla_gate_b, gla_norm,
                       ssd_conv_w, ssd_conv_b, ssd_dt_bias, ssd_a_log, ssd_d, ssd_norm,
                       rw_mu, rw_w0, rw_w2, rw_a0, rw_a2, rw_g2, rw_k_k, rw_k_a, rw_r_k, rw_norm_w, rw_norm_b,
                       w_branch, gate_b, w_out):
    B, S, _ = u.shape
    f32 = jnp.float32
    p = u @ w_in
    p_da, p_gla, p_ssd, p_rw, p_gate = split_cols(p, MIXER_COLS)

    q, k, v = split_cols(p_da, DA_SIZES)
    lam_init = 0.8 - 0.6 * math.exp(-0.3 * lidx)
    lam = (jnp.exp(jnp.sum(da_lambda_q1.astype(f32) * da_lambda_k1.astype(f32)))
           - jnp.exp(jnp.sum(da_lambda_q2.astype(f32) * da_lambda_k2.astype(f32))) + lam_init)
    y_a = differential_attention(q.reshape(B, S, DA_HEADS, 2, DA_QK_DIM), k.reshape(B, S, DA_HEADS, 2, DA_QK_DIM),
                                 v.reshape(B, S, DA_HEADS, DA_V_DIM), lam, lam_init, da_norm)

    q, k, v, og, glr = split_cols(p_gla, GLA_SIZES)
    log_g = jax.nn.log_sigmoid((glr @ gla_gate_w2 + gla_gate_b).astype(f32)) / GLA_GATE_TAU
    o = gla_chunked(q.reshape(B, S, GLA_HEADS, GLA_DK) * (GLA_DK ** -0.5), k.reshape(B, S, GLA_HEADS, GLA_DK),
                    v.reshape(B, S, GLA_HEADS, GLA_DV), log_g.reshape(B, S, GLA_HEADS, GLA_DK))
    y_b = rmsnorm(o, gla_norm).reshape(B, S, -1).astype(u.dtype) * jax.nn.silu(og)

    z, xbc, dt = split_cols(p_ssd, SSD_SIZES)
    xbc = jax.nn.silu(causal_depthwise_conv(xbc, ssd_conv_w, ssd_conv_b))
    xs, bm, cm = split_cols(xbc, (SSD_INNER, SSD_GROUPS * SSD_STATE, SSD_GROUPS * SSD_STATE))
    dt = jax.nn.softplus((dt + ssd_dt_bias).astype(f32))
    A = -jnp.exp(ssd_a_log.astype(f32))
    rep = SSD_HEADS // SSD_GROUPS
    bm = jnp.repeat(bm.reshape(B, S, SSD_GROUPS, SSD_STATE), rep, axis=2)
    cm = jnp.repeat(cm.reshape(B, S, SSD_GROUPS, SSD_STATE), rep, axis=2)
    xh = xs.reshape(B, S, SSD_HEADS, SSD_HEAD_DIM)
    y = ssd_chunked(xh, dt, A, bm, cm) + ssd_d.astype(f32)[:, None] * xh.astype(f32)
    y = y.reshape(B, S, SSD_INNER) * jax.nn.silu(z.astype(f32))
    y_c = rmsnorm(y.reshape(B, S, SSD_GROUPS, -1), ssd_norm.reshape(SSD_GROUPS, -1)).reshape(B, S, SSD_INNER)
    y_c = y_c.astype(u.dtype)

    p_rw = p_rw + (token_shift(p_rw) - p_rw) * rw_mu
    r, k, v, wlr, alr, glr = split_cols(p_rw, RW_SIZES)
    log_w = -RW_DECAY_SCALE * jax.nn.sigmoid((rw_w0 + jnp.tanh(wlr) @ rw_w2).astype(f32))
    a = jax.nn.sigmoid((rw_a0 + alr @ rw_a2).astype(f32))
    g = jax.nn.sigmoid(glr) @ rw_g2
    hs = (B, S, RW_HEADS, RW_HEAD)
    kk = (k.astype(f32) * rw_k_k.astype(f32)).reshape(hs)
    kk = kk / jnp.maximum(jnp.sqrt(jnp.sum(kk * kk, axis=-1, keepdims=True)), 1e-12)
    k = k.astype(f32) * (1.0 + (a - 1.0) * rw_k_a.astype(f32))
    r4, k4, v4, a4 = r.astype(f32).reshape(hs), k.reshape(hs), v.astype(f32).reshape(hs), a.reshape(hs)
    y = rwkv7_recurrence(r4, jnp.exp(log_w).reshape(hs), k4, v4, kk, a4)
    mu = jnp.mean(y, axis=-1, keepdims=True)
    var = jnp.mean(jnp.square(y - mu), axis=-1, keepdims=True)
    y = ((y - mu) * lax.rsqrt(var + RW_GN_EPS)).reshape(B, S, RW_DIM) * rw_norm_w.astype(f32) + rw_norm_b.astype(f32)
    bonus = jnp.sum(r4 * k4 * rw_r_k.astype(f32).reshape(RW_HEADS, RW_HEAD), axis=-1, keepdims=True) * v4
    y_d = ((y + bonus.reshape(B, S, RW_DIM)) * g.astype(f32)).astype(u.dtype)

    ys = jnp.stack([y_a.astype(u.dtype), y_b.astype(u.dtype), y_c, y_d], axis=2)
    proj = jnp.einsum('bsnc,ncd->bsnd', ys, w_branch)
    gates = jax.nn.sigmoid(p_gate.reshape(B, S, N_BRANCH, D_MODEL) + gate_b)
    merged = jnp.sum(gates * proj, axis=2)
    return merged @ w_out


def setup_inputs(seed: int = 0) -> dict:
    key = jax.random.key(seed)
    ks = iter(jax.random.split(key, 64))
    L, D, F = DEPTH, D_MODEL, D_FF

    def nrm(shape, scale):
        return jax.random.normal(next(ks), shape, jnp.float32) * scale

    def gain(shape):
        return 1.0 + nrm(shape, 0.01)

    x = nrm((BATCH, SEQ, D), 1.0)
    ffn1_norm = gain((L, D))
    ffn1_wg = nrm((L, D, F), D ** -0.5)
    ffn1_wu = nrm((L, D, F), D ** -0.5)
    ffn1_wd = nrm((L, F, D), F ** -0.5)
    mix_norm = gain((L, D))
    w_in = nrm((L, D, N_IN), D ** -0.5)
    da_lambda_q1 = nrm((L, DA_QK_DIM), 0.1)
    da_lambda_k1 = nrm((L, DA_QK_DIM), 0.1)
    da_lambda_q2 = nrm((L, DA_QK_DIM), 0.1)
    da_lambda_k2 = nrm((L, DA_QK_DIM), 0.1)
    da_norm = gain((L, DA_V_DIM))
    gla_gate_w2 = nrm((L, GLA_GATE_RANK, GLA_HEADS * GLA_DK), GLA_GATE_RANK ** -0.5)
    gla_gate_b = nrm((L, GLA_HEADS * GLA_DK), 0.1)
    gla_norm = gain((L, GLA_DV))
    ssd_conv_w = nrm((L, SSD_CONV, SSD_CONV_DIM), SSD_CONV ** -0.5)
    ssd_conv_b = nrm((L, SSD_CONV_DIM), 0.01)
    dt0 = jnp.exp(jax.random.uniform(next(ks), (L, SSD_HEADS), jnp.float32, math.log(1e-3), math.log(1e-1)))
    ssd_dt_bias = dt0 + jnp.log(-jnp.expm1(-dt0))
    ssd_a_log = jnp.log(jax.random.uniform(next(ks), (L, SSD_HEADS), jnp.float32, 1.0, 16.0))
    ssd_d = gain((L, SSD_HEADS))
    ssd_norm = gain((L, SSD_INNER))
    rw_mu = jax.random.uniform(next(ks), (L, sum(RW_SIZES)), jnp.float32)
    rw_w0 = nrm((L, RW_DIM), 0.5)
    rw_w2 = nrm((L, RW_DECAY_RANK, RW_DIM), RW_DECAY_RANK ** -0.5)
    rw_a0 = nrm((L, RW_DIM), 0.1)
    rw_a2 = nrm((L, RW_A_RANK, RW_DIM), RW_A_RANK ** -0.5)
    rw_g2 = nrm((L, RW_GATE_RANK, RW_DIM), RW_GATE_RANK ** -0.5)
    rw_k_k = 0.85 + nrm((L, RW_DIM), 0.01)
    rw_k_a = gain((L, RW_DIM))
    rw_r_k = nrm((L, RW_DIM), 0.1)
    rw_norm_w = gain((L, RW_DIM))
    rw_norm_b = nrm((L, RW_DIM), 0.01)
    w_branch = nrm((L, N_BRANCH, MIX_WIDTH, D), MIX_WIDTH ** -0.5)
    gate_b = nrm((L, N_BRANCH, D), 0.1)
    w_out = nrm((L, D, D), D ** -0.5)
    ffn2_norm = gain((L, D))
    ffn2_wg = nrm((L, D, F), D ** -0.5)
    ffn2_wu = nrm((L, D, F), D ** -0.5)
    ffn2_wd = nrm((L, F, D), F ** -0.5)
    final_norm = gain((D,))
    return {
        "x": x, "ffn1_norm": ffn1_norm, "ffn1_wg": ffn1_wg, "ffn1_wu": ffn1_wu, "ffn1_wd": ffn1_wd,
        "mix_norm": mix_norm, "w_in": w_in,
        "da_lambda_q1": da_lambda_q1, "da_lambda_k1": da_lambda_k1, "da_lambda_q2": da_lambda_q2,
        "da_lambda_k2": da_lambda_k2, "da_norm": da_norm,
        "gla_gate_w2": gla_gate_w2, "gla_gate_b": gla_gate_b, "gla_norm": gla_norm,
        "ssd_conv_w": ssd_conv_w, "ssd_conv_b": ssd_conv_b, "ssd_dt_bias": ssd_dt_bias, "ssd_a_log": ssd_a_log,
        "ssd_d": ssd_d, "ssd_norm": ssd_norm,
        "rw_mu": rw_mu, "rw_w0": rw_w0, "rw_w2": rw_w2, "rw_a0": rw_a0, "rw_a2": rw_a2, "rw_g2": rw_g2,
        "rw_k_k": rw_k_k, "rw_k_a": rw_k_a, "rw_r_k": rw_r_k, "rw_norm_w": rw_norm_w, "rw_norm_b": rw_norm_b,
        "w_branch": w_branch, "gate_b": gate_b, "w_out": w_out,
        "ffn2_norm": ffn2_norm, "ffn2_wg": ffn2_wg, "ffn2_wu": ffn2_wu, "ffn2_wd": ffn2_wd,
        "final_norm": final_norm,
    }


def reference(x, ffn1_norm, ffn1_wg, ffn1_wu, ffn1_wd, mix_norm, w_in,
              da_lambda_q1, da_lambda_k1, da_lambda_q2, da_lambda_k2, da_norm,
              gla_gate_w2, gla_gate_b, gla_norm,
              ssd_conv_w, ssd_conv_b, ssd_dt_bias, ssd_a_log, ssd_d, ssd_norm,
              rw_mu, rw_w0, rw_w2, rw_a0, rw_a2, rw_g2, rw_k_k, rw_k_a, rw_r_k, rw_norm_w, rw_norm_b,
              w_branch, gate_b, w_out, ffn2_norm, ffn2_wg, ffn2_wu, ffn2_wd, final_norm):
    h = x
    for l in range(DEPTH):
        h = h + 0.5 * swiglu_ffn(rmsnorm(h, ffn1_norm[l]), ffn1_wg[l], ffn1_wu[l], ffn1_wd[l])
        h = h + hybrid_token_mixer(
            rmsnorm(h, mix_norm[l]), l, w_in[l],
            da_lambda_q1[l], da_lambda_k1[l], da_lambda_q2[l], da_lambda_k2[l], da_norm[l],
            gla_gate_w2[l], gla_gate_b[l], gla_norm[l],
            ssd_conv_w[l], ssd_conv_b[l], ssd_dt_bias[l], ssd_a_log[l], ssd_d[l], ssd_norm[l],
            rw_mu[l], rw_w0[l], rw_w2[l], rw_a0[l], rw_a2[l], rw_g2[l], rw_k_k[l], rw_k_a[l], rw_r_k[l],
            rw_norm_w[l], rw_norm_b[l], w_branch[l], gate_b[l], w_out[l])
        h = h + 0.5 * swiglu_ffn(rmsnorm(h, ffn2_norm[l]), ffn2_wg[l], ffn2_wu[l], ffn2_wd[l])
    return rmsnorm(h, final_norm)
```

```python
import math
import numpy as np
import ml_dtypes
from concourse.bass_utils import run_bass_kernel_spmd

import numpy as np
import concourse.bass as bass
import concourse.mybir as mybir
from contextlib import ExitStack

F32 = mybir.dt.float32
BF16 = mybir.dt.bfloat16
AF = mybir.ActivationFunctionType
ALU = mybir.AluOpType
AX = mybir.AxisListType

ENGS = ("pe", "act", "dve", "pool", "sp")


class Buf:
    __slots__ = ("name", "t", "last_w", "readers")

    def __init__(self, name, t):
        self.name = name
        self.t = t
        self.last_w = []
        self.readers = []

    def __getitem__(self, k):
        return self.t[k]


class SubBuf:
    def __init__(self, parent, ap, name=None):
        self.parent = parent
        self.t = ap
        self.name = name or parent.name

    @property
    def last_w(self):
        return self.parent.last_w

    @last_w.setter
    def last_w(self, v):
        self.parent.last_w = v

    @property
    def readers(self):
        return self.parent.readers

    @readers.setter
    def readers(self, v):
        self.parent.readers = v

    def __getitem__(self, k):
        return self.t[k]


class Sched:
    def __init__(self, nc, n_dma_sems=24):
        self.nc = nc
        self.es = ExitStack()
        self.q = {e: [] for e in ENGS}
        self.cnt = {e: 0 for e in ENGS}
        self.sem = {e: self.es.enter_context(nc.semaphore("s_" + e)) for e in ENGS if e != "sp"}
        self.seen = {e: {} for e in ENGS}
        self.dsem = [self.es.enter_context(nc.semaphore("d%d" % i)) for i in range(n_dma_sems)]
        self.dval = [0] * n_dma_sems
        self.dnext = 0
        self.dnext_pool = 0
        self.nbuf = 0

    def sb(self, shape, dt, name=None):
        self.nbuf += 1
        name = name or ("sb%d" % self.nbuf)
        t = self.es.enter_context(self.nc.sbuf_tensor(name, list(shape), dt))
        return Buf(name, t)

    def ps(self, shape, dt=F32, name=None):
        self.nbuf += 1
        name = name or ("ps%d" % self.nbuf)
        t = self.es.enter_context(self.nc.psum_tensor(name, list(shape), dt))
        return Buf(name, t)

    def dram(self, name, shape, dt, kind):
        t = self.nc.dram_tensor(name, list(shape), dt, kind=kind)
        return Buf(name, t.ap())

    def view(self, name, ap):
        return Buf(name, ap)

    def _wait(self, eng, dep):
        if dep is None:
            return
        if dep[0] == "dma":
            _, si, val = dep
            key = ("d", si)
            sem = self.dsem[si]
        else:
            e2, val = dep
            if e2 == "pe" and eng == "pe":
                return
            key = e2
            sem = self.sem[e2]
        if self.seen[eng].get(key, 0) >= val:
            return
        self.seen[eng][key] = val
        self.q[eng].append(("wait", sem, val))

    def _deps(self, eng, reads, writes, partial=False):
        for b in reads:
            for w in b.last_w:
                self._wait(eng, w)
        for b in writes:
            if not partial:
                for w in b.last_w:
                    self._wait(eng, w)
            for r in b.readers:
                self._wait(eng, r)

    def op(self, eng, fn, reads=(), writes=(), *args, **kw):
        if isinstance(fn, str):
            name = fn
            fn = (lambda e, name=name, args=args, kw=kw: getattr(e, name)(*args, **kw))
        self._deps(eng, reads, writes)
        self.cnt[eng] += 1
        idx = self.cnt[eng]
        self.q[eng].append(("op", fn, self.sem[eng]))
        me = (eng, idx)
        for b in reads:
            b.readers.append(me)
        for b in writes:
            b.last_w = [me]
            b.readers = []
        return me

    def dma(self, eng, out_ap, in_ap, reads=(), writes=(), partial=False, **kw):
        self._deps(eng, reads, writes, partial)
        half = len(self.dsem) // 2
        if eng == "pool":
            si = half + self.dnext_pool
            self.dnext_pool = (self.dnext_pool + 1) % half
        else:
            si = self.dnext
            self.dnext = (self.dnext + 1) % half
        if self.dval[si] > 0:
            self._wait(eng, ("dma", si, self.dval[si]))
        self.dval[si] += 16
        val = self.dval[si]
        sem = self.dsem[si]
        self.q[eng].append(("dma", out_ap, in_ap, sem, kw))
        me = ("dma", si, val)
        for b in reads:
            b.readers.append(me)
        for b in writes:
            if partial:
                b.last_w = b.last_w + [me]
            else:
                b.last_w = [me]
                b.readers = []
        return me

    def barrier(self):
        for e in ENGS:
            for e2 in ENGS:
                if e2 != "sp" and self.cnt[e2] > 0:
                    self._wait(e, (e2, self.cnt[e2]))
            for si in range(len(self.dsem)):
                if self.dval[si] > 0:
                    self._wait(e, ("dma", si, self.dval[si]))

    def finish_wait(self, eng, bufs=None):
        for si in range(len(self.dsem)):
            if self.dval[si] > 0:
                self._wait(eng, ("dma", si, self.dval[si]))

    def emit(self):
        nc = self.nc
        q = self.q

        def run(engobj, lst):
            for it in lst:
                if it[0] == "wait":
                    engobj.wait_ge(it[1], it[2])
                elif it[0] == "op":
                    it[1](engobj).then_inc(it[2], 1)
                else:
                    _, o, i, sem, kw = it
                    engobj.dma_start(out=o, in_=i, **kw).then_inc(sem, 16)

        with nc.Block() as block:
            @block.tensor
            def _(e):
                run(e, q["pe"])

            @block.scalar
            def _(e):
                run(e, q["act"])

            @block.vector
            def _(e):
                run(e, q["dve"])

            @block.gpsimd
            def _(e):
                run(e, q["pool"])

            @block.sync
            def _(e):
                run(e, q["sp"])
        self.es.close()


D = 1024
TT = 512
FF = 2816
FG = 256
NFG = FF // FG
EPS = 1e-6
NBR = 4
MW = 512


def build_tok(do_merge, do_ffn1, do_final, NT=2048):
    NTT = NT // TT
    nc = bass.Bass("TRN2", target_bir_lowering=False)
    S = Sched(nc)
    hT_in = S.dram("hT_in", [D, NT], F32, "ExternalInput")
    hT_out = S.dram("hT_out", [D, NT], F32, "ExternalOutput")
    hT = S.sb([128, 8, NT], F32, "hT")
    arenaA = S.sb([128, max(8 * NT, 32 * TT)], BF16, "arenaA")
    WSZ = 8 * FG
    arenaW = S.sb([128, 6 * WSZ], BF16, "arenaW")
    ones = S.sb([128, 128], F32, "ones")
    epsb = S.sb([128, 1], F32, "epsb")
    sq = [S.sb([128, TT], F32, "sq%d" % i) for i in range(2)]
    rstd = S.sb([128, TT], F32, "rstd")
    tmpf = [S.sb([128, TT], F32, "tmpf%d" % i) for i in range(2)]
    abuf = [[S.sb([128, TT], BF16, "abuf%d_%d" % (i, c)) for c in range(2)] for i in range(2)]
    psA = [S.ps([128, TT], F32, "psA%d" % i) for i in range(4)]
    psO = [S.ps([128, TT], F32, "psO%d" % i) for i in range(2)]
    psN = S.ps([128, TT], F32, "psN")
    cnt = {"a": 0, "o": 0, "t": 0}

    def nxt(lst, key):
        b = lst[cnt[key] % len(lst)]
        cnt[key] += 1
        return b

    S.op("pool", "memset", [], [ones], ones[:], 1.0 / D)
    S.op("pool", "memset", [], [epsb], epsb[:], EPS)
    for k in range(8):
        S.dma("sp", hT[:, k, :], hT_in[k * 128:(k + 1) * 128, :], writes=[hT], partial=(k > 0))

    def load_small(name, shape):
        gd = S.dram(name, list(shape), F32, "ExternalInput")
        g = S.sb(list(shape), F32, name + "_sb")
        S.dma("sp", g[:], gd[:], writes=[g])
        return g

    unT = arenaA[:, 0:8 * NT].rearrange("p (k t) -> p k t", t=NT)

    def norm(g, out_f32=False):
        for t in range(NTT):
            ts = slice(t * TT, (t + 1) * TT)
            for k in range(8):
                s_ = sq[k % 2]
                S.op("act", "activation", [hT], [s_], out=s_[:], in_=hT[:, k, ts], func=AF.Square)
                S.op("pe", "matmul", [ones, s_], [psN], out=psN[:], lhsT=ones[:], rhs=s_[:], start=(k == 0), stop=(k == 7))
            S.op("act", "activation", [psN, epsb], [rstd], out=rstd[:], in_=psN[:], func=AF.Sqrt, bias=epsb[:], scale=1.0)
            S.op("dve", "reciprocal", [rstd], [rstd], out=rstd[:], in_=rstd[:])
            for k in range(8):
                if out_f32:
                    S.op("dve", "scalar_tensor_tensor", [hT, g, rstd], [hT], out=hT[:, k, ts], in0=hT[:, k, ts], scalar=g[:, k:k + 1],
                         in1=rstd[:], op0=ALU.mult, op1=ALU.mult)
                else:
                    S.op("dve", "scalar_tensor_tensor", [hT, g, rstd], [arenaA], out=unT[:, k, ts], in0=hT[:, k, ts], scalar=g[:, k:k + 1],
                         in1=rstd[:], op0=ALU.mult, op1=ALU.mult)

    def ffn_stage(pfx):
        g = load_small(pfx + "_g", [128, 8])
        wg = S.dram(pfx + "_wg", [D, FF], F32, "ExternalInput")
        wu = S.dram(pfx + "_wu", [D, FF], F32, "ExternalInput")
        wd = S.dram(pfx + "_wd", [FF, D], F32, "ExternalInput")
        norm(g)
        slots = []
        for i in range(2):
            base = i * 3 * WSZ
            wgb = arenaW[:, base:base + WSZ].rearrange("p (k f) -> p k f", f=FG)
            wub = arenaW[:, base + WSZ:base + 2 * WSZ].rearrange("p (k f) -> p k f", f=FG)
            wdb = arenaW[:, base + 2 * WSZ:base + 3 * WSZ].rearrange("p (c d) -> p c d", d=D)
            slots.append((S.view("wg%d" % i, wgb), S.view("wu%d" % i, wub), S.view("wd%d" % i, wdb)))
        wg_v = wg[:].rearrange("(k p) f -> p k f", p=128)
        wu_v = wu[:].rearrange("(k p) f -> p k f", p=128)
        wd_v = wd[:].rearrange("(c p) d -> p c d", p=128)
        for gi in range(NFG):
            bg, bu, bd = slots[gi % 2]
            for k in range(0, 8, 2):
                S.dma("pool", bg.t[:, k:k + 2, :], wg_v[:, k:k + 2, gi * FG:(gi + 1) * FG], writes=[bg], partial=(k > 0))
                S.dma("pool", bu.t[:, k:k + 2, :], wu_v[:, k:k + 2, gi * FG:(gi + 1) * FG], writes=[bu], partial=(k > 0))
            S.dma("pool", bd.t, wd_v[:, 2 * gi:2 * gi + 2, :], writes=[bd])
            for t in range(NTT):
                ts = slice(t * TT, (t + 1) * TT)
                ab = abuf[cnt["t"] % 2]
                cnt["t"] += 1
                for c in range(2):
                    pg = nxt(psA, "a")
                    pu = nxt(psA, "a")
                    for k in range(8):
                        S.op("pe", "matmul", [bg, arenaA], [pg], out=pg[:], lhsT=bg.t[:, k, c * 128:(c + 1) * 128], rhs=unT[:, k, ts],
                             start=(k == 0), stop=(k == 7))
                    for k in range(8):
                        S.op("pe", "matmul", [bu, arenaA], [pu], out=pu[:], lhsT=bu.t[:, k, c * 128:(c + 1) * 128], rhs=unT[:, k, ts],
                             start=(k == 0), stop=(k == 7))
                    tf = tmpf[c]
                    S.op("act", "activation", [pg], [tf], out=tf[:], in_=pg[:], func=AF.Silu)
                    S.op("dve", "tensor_tensor", [tf, pu], [ab[c]], out=ab[c][:], in0=tf[:], in1=pu[:], op=ALU.mult)
                for dc in range(8):
                    po = nxt(psO, "o")
                    for c in range(2):
                        S.op("pe", "matmul", [bd, ab[c]], [po], out=po[:], lhsT=bd.t[:, c, dc * 128:(dc + 1) * 128], rhs=ab[c][:],
                             start=(c == 0), stop=(c == 1))
                    S.op("dve", "scalar_tensor_tensor", [po, hT], [hT], out=hT[:, dc, ts], in0=po[:], scalar=0.5, in1=hT[:, dc, ts],
                         op0=ALU.mult, op1=ALU.add)

    def merge_stage():
        uT_in = S.dram("uT_in", [D, NT], BF16, "ExternalInput")
        yT_in = S.dram("yT_in", [NBR * MW, NT], BF16, "ExternalInput")
        wgate = S.dram("w_gate", [D, NBR * D], F32, "ExternalInput")
        wbr = S.dram("w_branch", [NBR * MW, D], F32, "ExternalInput")
        wout = S.dram("w_out", [D, D], F32, "ExternalInput")
        gb = load_small("gate_b", [128, NBR * 8])
        wo_sb = S.sb([128, 8, D], BF16, "wo_sb")
        wout_v = wout[:].rearrange("(k p) d -> p k d", p=128)
        for k in range(8):
            S.dma("pool", wo_sb[:, k, :], wout_v[:, k, :], writes=[wo_sb], partial=(k > 0))
        mrg = S.sb([128, 8, TT], F32, "mrg")
        yT_t = S.view("yT_t", arenaA[:, 0:16 * TT].rearrange("p (c t) -> p c t", t=TT))
        uT_t = S.view("uT_t", arenaA[:, 16 * TT:24 * TT].rearrange("p (k t) -> p k t", t=TT))
        mb = S.view("mb", arenaA[:, 24 * TT:32 * TT].rearrange("p (k t) -> p k t", t=TT))
        wgm = S.view("wgm", arenaW[:, 0:8 * D].rearrange("p (k d) -> p k d", d=D))
        wbm = S.view("wbm", arenaW[:, 8 * D:12 * D].rearrange("p (c d) -> p c d", d=D))
        wgate_v = wgate[:].rearrange("(k p) n -> p k n", p=128)
        wbr_v = wbr[:].rearrange("(c p) d -> p c d", p=128)
        yT_v = yT_in[:].rearrange("(c p) t -> p c t", p=128)
        uT_v = uT_in[:].rearrange("(k p) t -> p k t", p=128)
        for t in range(NTT):
            ts = slice(t * TT, (t + 1) * TT)
            for c in range(0, 16, 4):
                S.dma("sp", yT_t.t[:, c:c + 4, :], yT_v[:, c:c + 4, ts], writes=[yT_t], partial=(c > 0))
            for k in range(0, 8, 4):
                S.dma("sp", uT_t.t[:, k:k + 4, :], uT_v[:, k:k + 4, ts], writes=[uT_t], partial=(k > 0))
            for m in range(NBR):
                for k in range(8):
                    S.dma("pool", wgm.t[:, k, :], wgate_v[:, k, m * D:(m + 1) * D], writes=[wgm], partial=(k > 0))
                for c in range(4):
                    S.dma("pool", wbm.t[:, c, :], wbr_v[:, 4 * m + c, :], writes=[wbm], partial=(c > 0))
                for dc in range(8):
                    pg = nxt(psA, "a")
                    pp = nxt(psA, "a")
                    for k in range(8):
                        S.op("pe", "matmul", [wgm, uT_t], [pg], out=pg[:], lhsT=wgm.t[:, k, dc * 128:(dc + 1) * 128], rhs=uT_t.t[:, k, :],
                             start=(k == 0), stop=(k == 7))
                    for c in range(4):
                        S.op("pe", "matmul", [wbm, yT_t], [pp], out=pp[:], lhsT=wbm.t[:, c, dc * 128:(dc + 1) * 128], rhs=yT_t.t[:, 4 * m + c, :],
                             start=(c == 0), stop=(c == 3))
                    tf = tmpf[dc % 2]
                    S.op("act", "activation", [pg, gb], [tf], out=tf[:], in_=pg[:], func=AF.Sigmoid,
                         bias=gb[:, m * 8 + dc:m * 8 + dc + 1], scale=1.0)
                    if m == 0:
                        S.op("dve", "tensor_tensor", [tf, pp], [mrg], out=mrg[:, dc, :], in0=tf[:], in1=pp[:], op=ALU.mult)
                    else:
                        S.op("dve", "tensor_tensor", [tf, pp], [tf], out=tf[:], in0=tf[:], in1=pp[:], op=ALU.mult)
                        S.op("dve", "tensor_tensor", [tf, mrg], [mrg], out=mrg[:, dc, :], in0=mrg[:, dc, :], in1=tf[:], op=ALU.add)
            for k in range(8):
                S.op("act", "activation", [mrg], [mb], out=mb.t[:, k, :], in_=mrg[:, k, :], func=AF.Copy)
            for dc in range(8):
                po = nxt(psO, "o")
                for k in range(8):
                    S.op("pe", "matmul", [wo_sb, mb], [po], out=po[:], lhsT=wo_sb[:, k, dc * 128:(dc + 1) * 128], rhs=mb.t[:, k, :],
                         start=(k == 0), stop=(k == 7))
                S.op("dve", "tensor_tensor", [po, hT], [hT], out=hT[:, dc, ts], in0=hT[:, dc, ts], in1=po[:], op=ALU.add)

    if do_merge:
        merge_stage()
        S.barrier()
        ffn_stage("f2")
    if do_ffn1:
        if do_merge:
            S.barrier()
        ffn_stage("f1")
        uT_out = S.dram("uT_out", [D, NT], BF16, "ExternalOutput")
        g = load_small("mixn_g", [128, 8])
        S.barrier()
        norm(g)
        for k in range(8):
            S.dma("sp", uT_out[k * 128:(k + 1) * 128, :], unT[:, k, :], reads=[arenaA], writes=[uT_out], partial=True)
    if do_final:
        g = load_small("final_g", [128, 8])
        norm(g, out_f32=True)
    for k in range(8):
        S.dma("sp", hT_out[k * 128:(k + 1) * 128, :], hT[:, k, :], reads=[hT], writes=[hT_out], partial=True)
    S.finish_wait("sp")
    S.emit()
    return nc


D = 1024
ST = 512

DA0, GLA0, SSD0, RW0 = 0, 1536, 3088, 4632


def core_cols(g):
    import numpy as np
    gr = g // 2
    cols = []
    off = {}

    def add(name, idx):
        off[name] = (len(cols), len(idx))
        cols.extend(list(idx))

    def swap(idx):
        idx = np.asarray(idx)
        return np.concatenate([np.concatenate([idx[b + 32:b + 64], idx[b:b + 32]]) for b in range(0, len(idx), 64)])

    r = np.arange
    add("aq", DA0 + g * 128 + r(128))
    add("aqs", swap(DA0 + g * 128 + r(128)))
    add("ak", DA0 + 512 + g * 128 + r(128))
    add("aks", swap(DA0 + 512 + g * 128 + r(128)))
    add("gq", GLA0 + g * 64 + r(64))
    add("gk", GLA0 + 256 + g * 64 + r(64))
    add("gglr", GLA0 + 1536 + r(16))
    add("sx0", SSD0 + 512 + g * 128 + r(128))
    add("sx1", SSD0 + 512 + (g ^ 1) * 128 + r(128))
    add("sB", SSD0 + 512 + 512 + gr * 128 + r(128))
    add("sC", SSD0 + 512 + 768 + gr * 128 + r(128))
    add("rr", RW0 + g * 128 + r(128))
    add("rk", RW0 + 512 + g * 128 + r(128))
    add("rv", RW0 + 1024 + g * 128 + r(128))
    add("rwl", RW0 + 1536 + r(64))
    add("ral", RW0 + 1600 + r(64))
    add("rgl", RW0 + 1664 + r(128))
    add("T0", np.concatenate([DA0 + 1024 + g * 128 + r(128), GLA0 + 512 + g * 128 + r(128), GLA0 + 1024 + g * 128 + r(128),
                              GLA0 + 256 + g * 64 + r(64)]))
    add("T1", np.concatenate([SSD0 + g * 128 + r(128), SSD0 + (g ^ 1) * 128 + r(128),
                              SSD0 + 1536 + g * 2 + r(2), SSD0 + 1536 + (g ^ 1) * 2 + r(2)]))
    return np.asarray(cols), off


NCOL = len(core_cols(0)[0])
OFF = core_cols(0)[1]


class H:
    def __init__(self, S):
        self.S = S

    def MM(self, o, oap, l, lap, r, rap, st=True, sp=True):
        self.S.op("pe", "matmul", [l, r], [o], out=oap, lhsT=lap, rhs=rap, start=st, stop=sp, skip_group_check=True)

    def TR(self, o, oap, i, iap, ident, idap):
        self.S.op("pe", "transpose", [i, ident], [o], out=oap, in_=iap, identity=idap)

    def ACT(self, o, oap, i, iap, func, bias=None, scale=1.0, rd=(), wr=(), **kw):
        k = dict(out=oap, in_=iap, func=func, scale=scale)
        if bias is not None:
            k["bias"] = bias
        k.update(kw)
        self.S.op("act", "activation", [i] + list(rd), [o] + list(wr), **k)

    def TT(self, o, oap, a, aap, b, bap, op, eng="dve"):
        self.S.op(eng, "tensor_tensor", [a, b], [o], out=oap, in0=aap, in1=bap, op=op)

    def TS(self, o, oap, a, aap, s1, op0, s2=None, op1=None, rd=(), eng="dve"):
        k = dict(out=oap, in0=aap, scalar1=s1, scalar2=s2, op0=op0)
        if op1 is not None:
            k["op1"] = op1
        self.S.op(eng, "tensor_scalar", [a] + list(rd), [o], **k)

    def STT(self, o, oap, a, aap, sc, b, bap, op0, op1, rd=()):
        self.S.op("dve", "scalar_tensor_tensor", [a, b] + list(rd), [o], out=oap, in0=aap, scalar=sc, in1=bap, op0=op0, op1=op1)

    def CP(self, o, oap, i, iap, eng="dve"):
        if eng == "act":
            self.S.op("act", "activation", [i], [o], out=oap, in_=iap, func=AF.Copy)
        else:
            self.S.op(eng, "tensor_copy", [i], [o], out=oap, in_=iap)


C_ID, C_TRI, C_TRIG, C_NEGM, C_MASKA, C_I2, C_RWS, C_RWI, C_RWST, C_BD = range(10)
NCONST = 10


def make_consts():
    import numpy as np
    c = np.zeros((128, NCONST, 128), np.float32)
    j = np.arange(128)[:, None]
    i = np.arange(128)[None, :]
    c[:, C_ID] = (j == i)
    c[:, C_TRI] = (j <= i)
    c[:, C_TRIG] = (j <= i) * (-1.0 / 16.0)
    c[:, C_NEGM] = np.where(j > i, -30000.0, 0.0)
    c[:, C_MASKA] = ((j // 64) <= (i // 64))
    c[:, C_I2, :64] = (j % 64 == i[:, :64])
    same = (j // 64) == (i // 64)
    c[:, C_RWS] = same & ((j % 64) < (i % 64))
    c[:, C_RWI] = same & ((j % 64) <= (i % 64))
    c[:, C_RWST] = same & ((j % 64) > (i % 64))
    c[:, C_BD] = same
    return c.reshape(128, NCONST * 128)


def build_mix(S_len, lam_init, en=("a", "b", "c", "d")):
    nc = bass.Bass("TRN2", target_bir_lowering=False)
    S = Sched(nc)
    h = H(S)
    NST = S_len // ST
    NKT = S_len // 128
    UT = S.dram("UT", [D, S_len], BF16, "ExternalInput")
    Wd = S.dram("W", [D, NCOL], F32, "ExternalInput")
    consts_d = S.dram("consts", [128, NCONST * 128], F32, "ExternalInput")
    yT = S.dram("yT", [512, S_len], BF16, "ExternalOutput")

    W = S.sb([128, 8, NCOL], BF16, "Wsb")
    Wv = Wd[:].rearrange("(k p) n -> p k n", p=128)
    for k in range(8):
        S.dma("pool", W[:, k, :], Wv[:, k, :], writes=[W], partial=(k > 0))
    cst = S.sb([128, NCONST, 128], F32, "cst")
    S.dma("sp", cst[:].rearrange("p a b -> p (a b)"), consts_d[:], writes=[cst])
    cstb = S.sb([128, NCONST, 128], BF16, "cstb")
    S.op("dve", "tensor_copy", [cst], [cstb], out=cstb[:], in_=cst[:])
    ident = cst[:, C_ID, :]
    identb = cstb[:, C_ID, :]
    epsb = S.sb([128, 1], F32, "epsb")
    S.op("pool", "memset", [], [epsb], epsb[:], 1e-6)

    def small(name, shape):
        d_ = S.dram(name, list(shape), F32, "ExternalInput")
        t = S.sb(list(shape), F32, name + "_sb")
        S.dma("sp", t[:], d_[:], writes=[t])
        return t

    psF = [S.ps([128, ST], F32, "psF%d" % i) for i in range(2)]
    psT0 = S.ps([128, 512], F32, "psT0")
    psT1 = S.ps([128, 512], F32, "psT1")
    psW = [S.ps([128, 4, 128], F32, "psW%d" % i) for i in range(3)]
    psTR = S.ps([128, 8, 128], BF16, "psTR")
    trslots = [SubBuf(psTR, psTR[:, j, :]) for j in range(4)]
    slots = []
    for b_ in psW:
        for j in range(4):
            slots.append(SubBuf(b_, b_[:, j, :]))
    cnt = {"f": 0, "s": 0}

    def PF():
        cnt["f"] += 1
        return psF[cnt["f"] % 2]

    def PS():
        cnt["s"] += 1
        return slots[cnt["s"] % len(slots)]

    UTt = [S.sb([128, 8, ST], BF16, "UTt0")] * 2
    UTv = UT[:].rearrange("(k p) t -> p k t", p=128)

    def fm_proj(name, ut, dst, dst_ap, rows=None):
        o, n = OFF[name]
        p = PF()
        for k in range(8):
            h.MM(p, p[0:n, :], W, W[:, k, o:o + n], ut, ut[:, k, :], st=(k == 0), sp=(k == 7))
        h.CP(dst, dst_ap, p, p[0:n, :], eng="act")

    yst = [S.sb([128, ST], BF16, "yst%d" % i) for i in range(4)]

    if "b" in en:
        gW2 = small("gla_w2aug", [17, 64])
        gnb = small("gla_norm_bc", [128, 128])
        g_glr = S.sb([17, ST], F32, "g_glr")
        S.op("pool", "memset", [], [g_glr], g_glr[:], 1.0)
        g_qT = S.sb([64, ST], F32, "g_qT")
        g_kT = S.sb([64, ST], F32, "g_kT")
        g_S = S.sb([64, 128], F32, "g_S")
        g_Sb = S.sb([64, 128], BF16, "g_Sb")
        S.op("pool", "memset", [], [g_S], g_S[:], 0.0)
        S.op("pool", "memset", [], [g_Sb], g_Sb[:], 0.0)
        g_e1 = S.sb([128, 64], F32, "g_e1")
        g_l = S.sb([128, 64], F32, "g_l")
        g_eGT = S.sb([64, 128], F32, "g_eGT")
        g_enGT = S.sb([64, 128], F32, "g_enGT")
        g_enG = S.sb([128, 64], F32, "g_enG")
        g_dec = S.sb([64, 1], F32, "g_dec")
        g_qin = S.sb([64, 128], BF16, "g_qin")
        g_ktT = S.sb([64, 128], BF16, "g_ktT")
        g_kt = S.sb([128, 64], BF16, "g_kt")
        g_v = S.sb([128, 128], BF16, "g_v")
        g_att = S.sb([128, 128], BF16, "g_att")
        g_y = S.sb([128, 128], F32, "g_y")
        g_sq = S.sb([128, 128], F32, "g_sq")
        g_ss = S.sb([128, 1], F32, "g_ss")
        g_sg = S.sb([128, 128], F32, "g_sg")
        g_yb = S.sb([128, 128], BF16, "g_yb")


    if "c" in en:
        s_cw = small("ssd_convw", [128, 16])
        s_cb = small("ssd_convb", [128, 4])
        s_dtb = small("ssd_dtb_bc", [128, 4])
        s_alog = small("ssd_alog_bc", [128, 4])
        s_D = small("ssd_d_bc", [128, 4])
        s_nb = small("ssd_norm_bc", [128, 256])
        s_A = S.sb([128, 4], F32, "s_A")
        h.ACT(s_A, s_A[:], s_alog, s_alog[:], AF.Exp)
        h.TS(s_A, s_A[:], s_A, s_A[:], -1.0, ALU.mult)
        s_ones = S.sb([128, 128], F32, "s_ones")
        S.op("pool", "memset", [], [s_ones], s_ones[:], 1.0)
        s_hb = [S.sb([128, 3 + ST], F32, "s_hb%d" % i) for i in range(4)]
        for b_ in s_hb:
            S.op("pool", "memset", [], [b_], b_[:], 0.0)
        s_acc = S.sb([128, ST], F32, "s_acc")
        s_fm = [S.sb([128, ST], BF16, "s_fm%d" % i) for i in range(4)]
        s_x = S.sb([128, 256], BF16, "s_x")
        s_B = S.sb([128, 128], BF16, "s_B")
        s_dt = S.sb([128, 4], F32, "s_dt")
        s_dtA = S.sb([128, 4], F32, "s_dtA")
        s_nac = S.sb([128, 4], F32, "s_nac")
        s_eac = S.sb([128, 4], F32, "s_eac")
        s_cd = S.sb([128, 4], F32, "s_cd")
        s_sc = S.sb([128, 128], F32, "s_sc")
        s_bc = S.sb([128, 128], F32, "s_bc")
        s_LT = S.sb([128, 128], F32, "s_LT")
        s_scm = S.sb([128, 128], BF16, "s_scm")
        s_xdt = S.sb([128, 256], BF16, "s_xdt")
        s_xdec = S.sb([128, 256], BF16, "s_xdec")
        s_yi = S.sb([128, 256], F32, "s_yi")
        s_y = S.sb([128, 256], F32, "s_y")
        s_sz = S.sb([128, 256], F32, "s_sz")
        s_sq = S.sb([128, 256], F32, "s_sq")
        s_ss = S.sb([128, 1], F32, "s_ss")
        s_yc = S.sb([128, 128], BF16, "s_yc")
        s_h = S.sb([128, 256], F32, "s_h")
        s_hbf = S.sb([128, 256], BF16, "s_hbf")
        S.op("pool", "memset", [], [s_h], s_h[:], 0.0)
        S.op("pool", "memset", [], [s_hbf], s_hbf[:], 0.0)

    def ssd_prep(ut):
        for gi, name in enumerate(("sx0", "sx1", "sB", "sC")):
            hb = s_hb[gi]
            h.CP(hb, hb[:, 0:3], hb, hb[:, ST:ST + 3], eng="pool")
            fm_proj(name, ut, hb, hb[:, 3:3 + ST])
            h.TS(s_acc, s_acc[:], hb, hb[:, 0:ST], s_cw[:, gi * 4:gi * 4 + 1], ALU.mult, rd=[s_cw])
            for k in range(1, 4):
                h.STT(s_acc, s_acc[:], hb, hb[:, k:k + ST], s_cw[:, gi * 4 + k:gi * 4 + k + 1], s_acc, s_acc[:], ALU.mult, ALU.add, rd=[s_cw])
            h.ACT(s_fm[gi], s_fm[gi][:], s_acc, s_acc[:], AF.Silu, bias=s_cb[:, gi:gi + 1], rd=[s_cb])

    def tr_bf(dst, dst_ap, src, src_ap):
        cnt["tr"] = cnt.get("tr", 0) + 1
        p = trslots[cnt["tr"] % len(trslots)]
        h.TR(p, p[:, :], src, src_ap, cstb, identb)
        h.CP(dst, dst_ap, p, p[:, :])

    def ssd_step(ut, j):
        js = slice(j * 128, (j + 1) * 128)
        TRI = cst[:, C_TRI, :]
        tr_bf(s_x, s_x[:, 0:128], s_fm[0], s_fm[0][:, js])
        tr_bf(s_x, s_x[:, 128:256], s_fm[1], s_fm[1][:, js])
        tr_bf(s_B, s_B[:], s_fm[2], s_fm[2][:, js])
        h.TT(s_dt, s_dt[:], s_dtb, s_dtb[:], psT1, psT1[:, 256:260], ALU.add)
        h.ACT(s_dt, s_dt[:], s_dt, s_dt[:], AF.Exp)
        h.ACT(s_dt, s_dt[:], s_dt, s_dt[:], AF.Ln, bias=1.0)
        h.TT(s_dtA, s_dtA[:], s_dt, s_dt[:], s_A, s_A[:], ALU.mult)
        h.ACT(s_sz, s_sz[:], psT1, psT1[:, 0:256], AF.Silu)
        pa = PS()
        h.MM(pa, pa[:, 0:4], cst, TRI, s_dtA, s_dtA[:])
        h.TS(s_nac, s_nac[:], pa, pa[:, 0:4], -1.0, ALU.mult)
        h.ACT(s_eac, s_eac[:], pa, pa[:, 0:4], AF.Exp)
        for hp in range(2):
            pyi = PS()
            h.MM(pyi, pyi[:, :], s_fm[3], s_fm[3][:, js], s_hbf, s_hbf[:, hp * 128:(hp + 1) * 128])
            for q_ in range(2):
                hh = hp * 2 + q_
                h.ACT(s_yi, s_yi[:, hh * 64:(hh + 1) * 64], pyi, pyi[:, q_ * 64:(q_ + 1) * 64], AF.Copy, scale=s_eac[:, hh:hh + 1], rd=[s_eac])
        psc = PS()
        h.MM(psc, psc[:, :], s_fm[2], s_fm[2][:, js], s_fm[3], s_fm[3][:, js])
        h.CP(s_sc, s_sc[:], psc, psc[:, :], eng="act")
        for hh in range(4):
            hb_ = slice(hh * 64, (hh + 1) * 64)
            h.TS(s_bc, s_bc[:], s_ones, s_ones[:], s_dtA[:, hh:hh + 1], ALU.mult, rd=[s_dtA])
            pr = PS()
            h.MM(pr, pr[:, :], s_bc, s_bc[:], cst, TRI, st=True, sp=False)
            h.MM(pr, pr[:, :], cst, ident, cst, cst[:, C_NEGM, :], st=False, sp=True)
            h.ACT(s_LT, s_LT[:], pr, pr[:, :], AF.Exp, bias=s_nac[:, hh:hh + 1], rd=[s_nac])
            h.ACT(s_cd, s_cd[:, hh:hh + 1], pr, pr[:, 127:128], AF.Exp)
            h.TT(s_scm, s_scm[:], s_sc, s_sc[:], s_LT, s_LT[:], ALU.mult)
            h.TS(s_xdt, s_xdt[:, hb_], s_x, s_x[:, hb_], s_dt[:, hh:hh + 1], ALU.mult, rd=[s_dt])
            py = PS()
            h.MM(py, py[:, 0:64], s_scm, s_scm[:], s_xdt, s_xdt[:, hb_])
            h.TT(s_y, s_y[:, hb_], s_yi, s_yi[:, hb_], py, py[:, 0:64], ALU.add)
            h.STT(s_y, s_y[:, hb_], s_x, s_x[:, hb_], s_D[:, hh:hh + 1], s_y, s_y[:, hb_], ALU.mult, ALU.add, rd=[s_D])
            h.TS(s_xdec, s_xdec[:, hb_], s_xdt, s_xdt[:, hb_], s_LT[:, 127:128], ALU.mult, rd=[s_LT])
        for hp in range(2):
            pcs = PS()
            h.MM(pcs, pcs[:, :], s_B, s_B[:], s_xdec, s_xdec[:, hp * 128:(hp + 1) * 128])
            for q_ in range(2):
                hh = hp * 2 + q_
                hb_ = slice(hh * 64, (hh + 1) * 64)
                h.STT(s_h, s_h[:, hb_], s_h, s_h[:, hb_], s_cd[:, hh:hh + 1], pcs, pcs[:, q_ * 64:(q_ + 1) * 64], ALU.mult, ALU.add, rd=[s_cd])
        h.CP(s_hbf, s_hbf[:], s_h, s_h[:], eng="act")
        h.TT(s_y, s_y[:], s_y, s_y[:], s_sz, s_sz[:], ALU.mult)
        h.ACT(s_sq, s_sq[:], s_y, s_y[:], AF.Square, accum_out=s_ss[:], scale=(1.0 / 256.0) ** 0.5, wr=[s_ss])
        h.ACT(s_ss, s_ss[:], s_ss, s_ss[:], AF.Sqrt, bias=epsb[:], rd=[epsb])
        S.op("dve", "reciprocal", [s_ss], [s_ss], out=s_ss[:], in_=s_ss[:])
        h.STT(s_yc, s_yc[:], s_y, s_y[:, 0:128], s_ss[:, 0:1], s_nb, s_nb[:, 0:128], ALU.mult, ALU.mult, rd=[s_ss])
        put_out(s_yc, 2, j)

    if "a" in en:
        ropeC = S.dram("ropeC", [128, S_len], F32, "ExternalInput")
        ropeS = S.dram("ropeS", [128, S_len], F32, "ExternalInput")
        a_lq = [small("da_l%d" % i, [128, 64]) for i in range(4)]
        a_nb = small("da_norm_bc", [128, 128])
        lamc = small("lamc", [128, 2])
        h.TS(a_nb, a_nb[:], a_nb, a_nb[:], lamc[:, 0:1], ALU.mult, rd=[lamc])
        a_t64 = S.sb([128, 64], F32, "a_t64")
        a_l1 = S.sb([128, 1], F32, "a_l1")
        a_l2 = S.sb([128, 1], F32, "a_l2")
        a_nlam = S.sb([128, 1], F32, "a_nlam")
        h.TT(a_t64, a_t64[:], a_lq[0], a_lq[0][:], a_lq[1], a_lq[1][:], ALU.mult)
        S.op("dve", "reduce_sum", [a_t64], [a_l1], out=a_l1[:], in_=a_t64[:], axis=AX.X)
        h.TT(a_t64, a_t64[:], a_lq[2], a_lq[2][:], a_lq[3], a_lq[3][:], ALU.mult)
        S.op("dve", "reduce_sum", [a_t64], [a_l2], out=a_l2[:], in_=a_t64[:], axis=AX.X)
        h.ACT(a_l1, a_l1[:], a_l1, a_l1[:], AF.Exp)
        h.ACT(a_l2, a_l2[:], a_l2, a_l2[:], AF.Exp)
        h.TT(a_nlam, a_nlam[:], a_l2, a_l2[:], a_l1, a_l1[:], ALU.subtract)
        h.TS(a_nlam, a_nlam[:], a_nlam, a_nlam[:], lamc[:, 1:2], ALU.add, rd=[lamc])
        a_cos = S.sb([128, ST], F32, "a_cos")
        a_sin = S.sb([128, ST], F32, "a_sin")
        a_f = [S.sb([128, ST], F32, "a_f%d" % i) for i in range(2)]
        a_qT = S.sb([128, ST], BF16, "a_qT")
        a_kT = S.sb([128, S_len], BF16, "a_kT")
        a_V = S.sb([128, NKT, 130], BF16, "a_V")
        S.op("pool", "memset", [], [a_V], a_V[:], 1.0)
        a_pT = [S.sb([128, ST], BF16, "a_pT%d" % i) for i in range(4)]
        a_z = S.sb([1, 512], BF16, "a_z")
        S.op("pool", "memset", [], [a_z], a_z[:], 0.0)
        a_o = S.sb([128, 128], F32, "a_o")
        a_r = S.sb([128, 2], F32, "a_r")
        a_sq = S.sb([128, 128], F32, "a_sq")
        a_ss = S.sb([128, 1], F32, "a_ss")
        a_y = S.sb([128, 128], BF16, "a_y")
        accflat = [b_[:].rearrange("p a b -> p (a b)") for b_ in psW]

    def att_prep(ut, t0):
        S.dma("sp", a_cos[:], ropeC[:, t0:t0 + ST], writes=[a_cos])
        S.dma("sp", a_sin[:], ropeS[:, t0:t0 + ST], writes=[a_sin])
        for (n0, n1, dst, dap) in (("aq", "aqs", a_qT, a_qT[:]), ("ak", "aks", a_kT, a_kT[:, t0:t0 + ST])):
            x0, x1 = a_f[0], a_f[1]
            fm_proj(n0, ut, x0, x0[:])
            fm_proj(n1, ut, x1, x1[:])
            h.TT(x0, x0[:], x0, x0[:], a_cos, a_cos[:], ALU.mult)
            h.TT(x1, x1[:], x1, x1[:], a_sin, a_sin[:], ALU.mult)
            h.TT(dst, dap, x0, x0[:], x1, x1[:], ALU.add)

    def att_run(st_i):
        for b_i in range(3):
            h.MM(psW[b_i], accflat[b_i][:, 0:390], a_z, a_z[0:1, 0:128], a_z, a_z[0:1, 0:390], st=True, sp=True)
        nkt = 4 * st_i + 4
        ci = 0
        for kt in range(nkt):
            ktl = kt - 4 * st_i
            qb0 = max(0, ktl)
            nq0 = qb0 * 128
            for c in range(2):
                sT = PF()
                cs = slice(c * 64, (c + 1) * 64)
                h.MM(sT, sT[:, nq0:ST], a_kT, a_kT[cs, kt * 128:(kt + 1) * 128], a_qT, a_qT[cs, nq0:ST])
                pT = a_pT[ci % 4]
                ci += 1
                h.ACT(pT, pT[:, nq0:ST], sT, sT[:, nq0:ST], AF.Exp, scale=0.125)
                if ktl >= 0:
                    h.TT(pT, pT[:, nq0:nq0 + 128], pT, pT[:, nq0:nq0 + 128], cstb, cstb[:, C_MASKA, :], ALU.mult)
                for qb in range(qb0, 4):
                    idx = qb * 2 + c
                    bk, of = idx // 3, (idx % 3) * 130
                    h.MM(psW[bk], accflat[bk][:, of:of + 130], pT, pT[:, qb * 128:(qb + 1) * 128], a_V, a_V[:, kt, :], st=False,
                         sp=(kt == 4 * st_i + qb))
        for qb in range(4):
            i1, i2 = qb * 2, qb * 2 + 1
            b1, o1 = i1 // 3, (i1 % 3) * 130
            b2, o2 = i2 // 3, (i2 % 3) * 130
            S.op("dve", "reciprocal", [psW[b1]], [a_r], out=a_r[:, 0:1], in_=accflat[b1][:, o1 + 128:o1 + 129])
            S.op("dve", "reciprocal", [psW[b2], a_r], [a_r], out=a_r[:, 1:2], in_=accflat[b2][:, o2 + 128:o2 + 129])
            h.TT(a_r, a_r[:, 1:2], a_r, a_r[:, 1:2], a_nlam, a_nlam[:], ALU.mult)
            h.TS(a_o, a_o[:], psW[b1], accflat[b1][:, o1:o1 + 128], a_r[:, 0:1], ALU.mult, rd=[a_r])
            h.STT(a_o, a_o[:], psW[b2], accflat[b2][:, o2:o2 + 128], a_r[:, 1:2], a_o, a_o[:], ALU.mult, ALU.add, rd=[a_r])
            h.ACT(a_sq, a_sq[:], a_o, a_o[:], AF.Square, accum_out=a_ss[:], scale=(1.0 / 128.0) ** 0.5, wr=[a_ss])
            h.ACT(a_ss, a_ss[:], a_ss, a_ss[:], AF.Sqrt, bias=epsb[:], rd=[epsb])
            S.op("dve", "reciprocal", [a_ss], [a_ss], out=a_ss[:], in_=a_ss[:])
            h.STT(a_y, a_y[:], a_o, a_o[:], a_ss[:, 0:1], a_nb, a_nb[:], ALU.mult, ALU.mult, rd=[a_ss])
            put_out(a_y, 0, qb)

    if "d" in en:
        r_mu = small("rw_mu", [128, 6])
        r_par = small("rw_par", [128, 5])
        r_nw = small("rw_nw_st", [128, 64])
        r_nb = small("rw_nb_st", [128, 64])
        r_w2d = S.dram("rw_w2", [64, 128], F32, "ExternalInput")
        r_a2d = S.dram("rw_a2", [64, 128], F32, "ExternalInput")
        r_g2d = S.dram("rw_g2", [128, 128], F32, "ExternalInput")
        r_w2 = S.sb([64, 128], BF16, "r_w2"); S.dma("pool", r_w2[:], r_w2d[:], writes=[r_w2])
        r_a2 = S.sb([64, 128], BF16, "r_a2"); S.dma("pool", r_a2[:], r_a2d[:], writes=[r_a2])
        r_g2 = S.sb([128, 128], BF16, "r_g2"); S.dma("pool", r_g2[:], r_g2d[:], writes=[r_g2])
        r_eps2 = S.sb([128, 1], F32, "r_eps2")
        S.op("pool", "memset", [], [r_eps2], r_eps2[:], 64e-5)
        r_ones = S.sb([128, 64], F32, "r_ones")
        S.op("pool", "memset", [], [r_ones], r_ones[:], 1.0)
        RN = ("rr", "rk", "rv", "rwl", "ral", "rgl")
        RROWS = (128, 128, 128, 64, 64, 128)
        r_pb = [S.sb([128, 1 + ST], F32, "r_pb%d" % i) for i in range(6)]
        for b_ in r_pb:
            S.op("pool", "memset", [], [b_], b_[:], 0.0)
        r_mx = [S.sb([128, ST], F32, "r_mx%d" % i) for i in range(6)]
        r_tb = S.sb([128, ST], BF16, "r_tb")
        r_lw = S.sb([128, ST], F32, "r_lw")
        r_a = S.sb([128, ST], F32, "r_a")
        r_sgT = S.sb([128, ST], BF16, "r_sgT")
        r_kk = S.sb([128, ST], F32, "r_kk")
        r_t1 = S.sb([128, ST], F32, "r_t1")
        r_cl = S.sb([128, ST], F32, "r_cl")
        r_Ep = S.sb([128, ST], F32, "r_Ep")
        r_Em = S.sb([128, ST], F32, "r_Em")
        r_Epr = r_t1
        r_At = r_cl
        r_Bt = r_lw
        r_Kt = r_mx[1]
        r_Rt = r_mx[0]
        r_RK = S.sb([128, ST], F32, "r_RK")
        r_bAR = [S.sb([128, 256], F32, "r_bAR%d" % i) for i in range(2)]
        r_bB = [S.sb([128, 128], F32, "r_bB%d" % i) for i in range(2)]
        r_bK = [S.sb([128, 128], F32, "r_bK%d" % i) for i in range(2)]
        r_bRK = [S.sb([128, 128], F32, "r_bRK%d" % i) for i in range(2)]
        r_bV = [S.sb([128, 128], F32, "r_bV%d" % i) for i in range(2)]
        r_Yb = S.sb([128, 128], F32, "r_Yb")
        for b_ in r_bAR + r_bB + r_bK + r_bRK + r_bV + [r_Yb]:
            S.op("pool", "memset", [], [b_], b_[:], 0.0)
        r_X = [S.sb([128, 128], F32, "r_X%d" % i) for i in range(2)]
        r_Y = [S.sb([128, 128], F32, "r_Y%d" % i) for i in range(2)]
        r_P = [S.sb([128, 128], F32, "r_P%d" % i) for i in range(2)]
        r_RBT = S.sb([128, 128], F32, "r_RBT")
        r_MkT = S.sb([128, 128], F32, "r_MkT")
        r_RKT = S.sb([128, 128], F32, "r_RKT")
        r_Btok = S.sb([128, 128], F32, "r_Btok")
        r_Ktok = S.sb([128, 128], F32, "r_Ktok")
        r_sV = S.sb([128, 64], F32, "r_sV")
        r_sW = S.sb([128, 64], F32, "r_sW")
        r_sU = S.sb([128, 64], F32, "r_sU")
        r_ST = S.sb([128, 64], F32, "r_ST")
        S.op("pool", "memset", [], [r_ST], r_ST[:], 0.0)
        r_bs = S.sb([128, 2], F32, "r_bs")
        r_y = S.sb([128, 64], F32, "r_y")
        r_j = S.sb([128, 64], F32, "r_j")
        r_m = S.sb([128, 1], F32, "r_m")
        r_v = S.sb([128, 1], F32, "r_v")

    def rw_prep(ut):
        for gi, name in enumerate(RN):
            n = RROWS[gi]
            pb = r_pb[gi]
            h.CP(pb, pb[0:n, 0:1], pb, pb[0:n, ST:ST + 1], eng="pool")
            fm_proj(name, ut, pb, pb[0:n, 1:1 + ST])
            mx = r_mx[gi]
            h.TT(mx, mx[0:n, :], pb, pb[0:n, 0:ST], pb, pb[0:n, 1:1 + ST], ALU.subtract)
            h.STT(mx, mx[0:n, :], mx, mx[0:n, :], r_mu[0:n, gi:gi + 1], pb, pb[0:n, 1:1 + ST], ALU.mult, ALU.add, rd=[r_mu])
        rr, rk, rv, rwl, ral, rgl = r_mx
        h.ACT(r_tb, r_tb[0:64, :], rwl, rwl[0:64, :], AF.Tanh)
        p = PF()
        h.MM(p, p[:, :], r_w2, r_w2[:], r_tb, r_tb[0:64, :])
        h.ACT(r_lw, r_lw[:], p, p[:, :], AF.Sigmoid, bias=r_par[:, 0:1], rd=[r_par])
        h.TS(r_lw, r_lw[:], r_lw, r_lw[:], -0.606531, ALU.mult)
        h.CP(r_tb, r_tb[0:64, :], ral, ral[0:64, :], eng="act")
        p = PF()
        h.MM(p, p[:, :], r_a2, r_a2[:], r_tb, r_tb[0:64, :])
        h.ACT(r_a, r_a[:], p, p[:, :], AF.Sigmoid, bias=r_par[:, 1:2], rd=[r_par])
        h.ACT(r_sgT, r_sgT[:], rgl, rgl[:], AF.Sigmoid)
        h.TS(r_kk, r_kk[:], rk, rk[:], r_par[:, 2:3], ALU.mult, rd=[r_par])
        h.TT(r_t1, r_t1[:], r_kk, r_kk[:], r_kk, r_kk[:], ALU.mult)
        p = PF()
        h.MM(p, p[:, :], cst, cst[:, C_BD, :], r_t1, r_t1[:])
        h.ACT(r_t1, r_t1[:], p, p[:, :], AF.Sqrt)
        h.TS(r_t1, r_t1[:], r_t1, r_t1[:], 1e-12, ALU.max)
        S.op("dve", "reciprocal", [r_t1], [r_t1], out=r_t1[:], in_=r_t1[:])
        h.TT(r_kk, r_kk[:], r_kk, r_kk[:], r_t1, r_t1[:], ALU.mult)
        h.TS(r_t1, r_t1[:], r_a, r_a[:], -1.0, ALU.add, r_par[:, 3:4], ALU.mult, rd=[r_par])
        h.TS(r_t1, r_t1[:], r_t1, r_t1[:], 1.0, ALU.add)
        h.TT(rk, rk[:], rk, rk[:], r_t1, r_t1[:], ALU.mult)
        for c in range(ST // 64):
            cs = slice(c * 64, (c + 1) * 64)
            S.op("dve", "tensor_tensor_scan", [r_ones, r_lw], [r_cl], out=r_cl[:, cs], data0=r_ones[:, 0:64], data1=r_lw[:, cs],
                 initial=0.0, op0=ALU.mult, op1=ALU.add)
        h.ACT(r_Ep, r_Ep[:], r_cl, r_cl[:], AF.Exp)
        h.ACT(r_Em, r_Em[:], r_cl, r_cl[:], AF.Exp, scale=-1.0)
        h.TT(r_t1, r_t1[:], r_cl, r_cl[:], r_lw, r_lw[:], ALU.subtract)
        h.ACT(r_Epr, r_Epr[:], r_t1, r_t1[:], AF.Exp)
        h.STT(r_RK, r_RK[:], rr, rr[:], r_par[:, 4:5], rk, rk[:], ALU.mult, ALU.mult, rd=[r_par])
        h.STT(r_At, r_At[:], r_kk, r_kk[:], -1.0, r_Epr, r_Epr[:], ALU.mult, ALU.mult)
        h.TT(r_Bt, r_Bt[:], r_kk, r_kk[:], r_a, r_a[:], ALU.mult)
        h.TT(r_Bt, r_Bt[:], r_Bt, r_Bt[:], r_Em, r_Em[:], ALU.mult)
        h.TT(r_Kt, r_Kt[:], rk, rk[:], r_Em, r_Em[:], ALU.mult)
        h.TT(r_Rt, r_Rt[:], rr, rr[:], r_Ep, r_Ep[:], ALU.mult)

    def rw_chunk(c):
        cs = slice(c * 64, (c + 1) * 64)
        par = c % 2
        bAR, bB, bK, bRK, bV = r_bAR[par], r_bB[par], r_bK[par], r_bRK[par], r_bV[par]
        rv = r_mx[2]
        for hh in range(2):
            ps_ = slice(hh * 64, (hh + 1) * 64)
            h.CP(bAR, bAR[ps_, hh * 64:(hh + 1) * 64], r_At, r_At[ps_, cs], eng="pool")
            h.CP(bAR, bAR[ps_, 128 + hh * 64:128 + (hh + 1) * 64], r_Rt, r_Rt[ps_, cs], eng="pool")
            h.CP(bB, bB[ps_, ps_], r_Bt, r_Bt[ps_, cs], eng="pool")
            h.CP(bK, bK[ps_, ps_], r_Kt, r_Kt[ps_, cs], eng="pool")
            h.CP(bRK, bRK[ps_, ps_], r_RK, r_RK[ps_, cs], eng="pool")
            h.CP(bV, bV[ps_, ps_], rv, rv[ps_, cs], eng="pool")
        bA = bAR[:, 0:128]
        bR = bAR[:, 128:256]
        p = PS(); h.MM(p, p[:, :], bB, bB[:], bAR, bA)
        h.TT(r_Y[0], r_Y[0][:], p, p[:, :], cst, cst[:, C_RWS, :], ALU.mult)
        p = PS(); h.MM(p, p[:, :], bB, bB[:], bAR, bR)
        h.TT(r_RBT, r_RBT[:], p, p[:, :], cst, cst[:, C_RWI, :], ALU.mult)
        p = PS(); h.MM(p, p[:, :], bK, bK[:], bAR, bA)
        h.TT(r_MkT, r_MkT[:], p, p[:, :], cst, cst[:, C_RWS, :], ALU.mult)
        p = PS(); h.MM(p, p[:, :], bK, bK[:], bAR, bR)
        h.TT(r_RKT, r_RKT[:], p, p[:, :], cst, cst[:, C_RWI, :], ALU.mult)
        p = PS(); h.MM(p, p[:, :], bAR, bA, bB, bB[:])
        h.TT(r_X[0], r_X[0][:], p, p[:, :], cst, cst[:, C_RWST, :], ALU.mult)
        h.TT(r_P[0], r_P[0][:], r_Y[0], r_Y[0][:], cst, ident, ALU.add)
        xi, pi = 0, 0
        for lvl in range(5):
            Xc, Yc, Xn, Yn = r_X[xi], r_Y[xi], r_X[1 - xi], r_Y[1 - xi]
            p = PS(); h.MM(p, p[:, :], Yc, Yc[:], Xc, Xc[:])
            h.CP(Xn, Xn[:], p, p[:, :], eng="act")
            if lvl < 4:
                p = PS(); h.MM(p, p[:, :], Xc, Xc[:], Yc, Yc[:])
                h.CP(Yn, Yn[:], p, p[:, :], eng="act")
            Pc, Pn = r_P[pi], r_P[1 - pi]
            p = PS(); h.MM(p, p[:, :], Xn, Xn[:], Pc, Pc[:])
            h.TT(Pn, Pn[:], Pc, Pc[:], p, p[:, :], ALU.add)
            xi, pi = 1 - xi, 1 - pi
        TT_ = r_P[pi]
        p = PS(); h.TR(p, p[:, :], bB, bB[:], cst, ident)
        h.CP(r_Btok, r_Btok[:], p, p[:, :], eng="act")
        p = PS(); h.TR(p, p[:, :], bK, bK[:], cst, ident)
        h.CP(r_Ktok, r_Ktok[:], p, p[:, :], eng="act")
        p = PS(); h.MM(p, p[:, 0:64], bV, bV[:], cst, cst[:, C_I2, 0:64])
        h.CP(r_sV, r_sV[:], p, p[:, 0:64], eng="act")
        p = PS(); h.MM(p, p[:, 0:2], bRK, bRK[:], r_ones, r_ones[:, 0:2])
        h.CP(r_bs, r_bs[:], p, p[:, 0:2])
        p = PS()
        h.MM(p, p[:, 0:64], r_MkT, r_MkT[:], r_sV, r_sV[:], st=True, sp=False)
        h.MM(p, p[:, 0:64], bAR, bA, r_ST, r_ST[:], st=False, sp=True)
        h.CP(r_sW, r_sW[:], p, p[:, 0:64])
        p = PS()
        h.MM(p, p[:, 0:64], TT_, TT_[:], r_sW, r_sW[:])
        h.CP(r_sU, r_sU[:], p, p[:, 0:64])
        pY = PS()
        h.MM(pY, pY[:, 0:64], bAR, bR, r_ST, r_ST[:], st=True, sp=False)
        h.MM(pY, pY[:, 0:64], r_RBT, r_RBT[:], r_sU, r_sU[:], st=False, sp=False)
        h.MM(pY, pY[:, 0:64], r_RKT, r_RKT[:], r_sV, r_sV[:], st=False, sp=True)
        h.CP(r_y, r_y[:], pY, pY[:, 0:64], eng="act")
        p = PS()
        h.MM(p, p[:, 0:64], r_Btok, r_Btok[:], r_sU, r_sU[:], st=True, sp=False)
        h.MM(p, p[:, 0:64], r_Ktok, r_Ktok[:], r_sV, r_sV[:], st=False, sp=True)
        h.TT(r_ST, r_ST[:], r_ST, r_ST[:], p, p[:, 0:64], ALU.add)
        h.TS(r_ST, r_ST[:], r_ST, r_ST[:], r_Ep[:, c * 64 + 63:c * 64 + 64], ALU.mult, rd=[r_Ep])
        h.ACT(r_j, r_j[:], r_y, r_y[:], AF.Copy, accum_out=r_m[:], scale=1.0 / 64.0, wr=[r_m])
        h.TS(r_y, r_y[:], r_y, r_y[:], r_m[:, 0:1], ALU.subtract, rd=[r_m])
        h.ACT(r_j, r_j[:], r_y, r_y[:], AF.Square, accum_out=r_v[:], scale=0.125, wr=[r_v])
        h.ACT(r_v, r_v[:], r_v, r_v[:], AF.Sqrt, bias=r_eps2[:], rd=[r_eps2])
        S.op("dve", "reciprocal", [r_v], [r_v], out=r_v[:], in_=r_v[:])
        h.STT(r_y, r_y[:], r_y, r_y[:], r_v[:, 0:1], r_nw, r_nw[:], ALU.mult, ALU.mult, rd=[r_v])
        h.TT(r_y, r_y[:], r_y, r_y[:], r_nb, r_nb[:], ALU.add)
        h.STT(r_y, r_y[:], r_sV, r_sV[:], r_bs[:, 0:1], r_y, r_y[:], ALU.mult, ALU.add, rd=[r_bs])
        pg = PS()
        h.MM(pg, pg[0:64, 0:64], r_sgT, r_sgT[:, cs], r_g2, r_g2[:, 0:64])
        h.MM(pg, pg[64:128, 0:64], r_sgT, r_sgT[:, cs], r_g2, r_g2[:, 64:128])
        for hh in range(2):
            ps_ = slice(hh * 64, (hh + 1) * 64)
            h.TT(r_Yb, r_Yb[ps_, ps_], r_y, r_y[ps_, :], pg, pg[ps_, 0:64], ALU.mult)
        p = PS()
        h.MM(p, p[:, 0:64], r_Yb, r_Yb[:], cst, cst[:, C_I2, 0:64])
        h.CP(yst[3], yst[3][:, cs], p, p[:, 0:64])

    def tm_proj(ut, j):
        o, n = OFF["T0"]
        for k in range(8):
            h.MM(psT0, psT0[:, 0:n], ut, ut[:, k, j * 128:(j + 1) * 128], W, W[:, k, o:o + n], st=(k == 0), sp=(k == 7))
        if "c" in en:
            o, n = OFF["T1"]
            for k in range(8):
                h.MM(psT1, psT1[:, 0:n], ut, ut[:, k, j * 128:(j + 1) * 128], W, W[:, k, o:o + n], st=(k == 0), sp=(k == 7))

    def gla_step(ut, j):
        js = slice(j * 128, (j + 1) * 128)
        tri = cst[:, C_TRIG, :]
        pz = PS()
        h.MM(pz, pz[:, 0:64], g_glr, g_glr[:, js], gW2, gW2[:, :])
        h.ACT(g_e1, g_e1[:], pz, pz[:, 0:64], AF.Exp, scale=-1.0)
        h.ACT(g_l, g_l[:], g_e1, g_e1[:], AF.Ln, bias=1.0)
        pG = PS()
        h.MM(pG, pG[:, 0:64], cst, tri, g_l, g_l[:])
        pGT = PS()
        h.MM(pGT, pGT[0:64, :], g_l, g_l[:], cst, tri)
        h.ACT(g_eGT, g_eGT[:], pGT, pGT[0:64, :], AF.Exp)
        h.ACT(g_enGT, g_enGT[:], pGT, pGT[0:64, :], AF.Exp, scale=-1.0)
        h.ACT(g_enG, g_enG[:], pG, pG[:, 0:64], AF.Exp, scale=-1.0)
        h.CP(g_dec, g_dec[:], g_eGT, g_eGT[:, 127:128])
        h.STT(g_qin, g_qin[:], g_qT, g_qT[:, js], 0.125, g_eGT, g_eGT[:], ALU.mult, ALU.mult)
        h.TT(g_ktT, g_ktT[:], g_kT, g_kT[:, js], g_enGT, g_enGT[:], ALU.mult)
        h.TT(g_kt, g_kt[:], g_enG, g_enG[:], psT0, psT0[:, 384:448], ALU.mult)
        h.CP(g_v, g_v[:], psT0, psT0[:, 128:256], eng="act")
        h.ACT(g_sg, g_sg[:], psT0, psT0[:, 256:384], AF.Silu)
        pA = PS()
        h.MM(pA, pA[:, :], g_ktT, g_ktT[:], g_qin, g_qin[:])
        h.TT(g_att, g_att[:], pA, pA[:, :], cst, cst[:, C_TRI, :], ALU.mult)
        pY = PS()
        h.MM(pY, pY[:, :], g_att, g_att[:], g_v, g_v[:], st=True, sp=False)
        h.MM(pY, pY[:, :], g_qin, g_qin[:], g_Sb, g_Sb[:], st=False, sp=True)
        pK = PS()
        h.MM(pK, pK[0:64, :], g_kt, g_kt[:], g_v, g_v[:])
        h.TT(g_S, g_S[:], g_S, g_S[:], pK, pK[0:64, :], ALU.add)
        h.TS(g_S, g_S[:], g_S, g_S[:], g_dec[:, 0:1], ALU.mult, rd=[g_dec])
        h.CP(g_Sb, g_Sb[:], g_S, g_S[:], eng="act")
        h.ACT(g_sq, g_sq[:], pY, pY[:, :], AF.Square, accum_out=g_ss[:], scale=(1.0 / 128.0) ** 0.5, wr=[g_ss])
        h.ACT(g_ss, g_ss[:], g_ss, g_ss[:], AF.Sqrt, bias=epsb[:], rd=[epsb])
        S.op("dve", "reciprocal", [g_ss], [g_ss], out=g_ss[:], in_=g_ss[:])
        h.STT(g_y, g_y[:], pY, pY[:, :], g_ss[:, 0:1], gnb, gnb[:], ALU.mult, ALU.mult, rd=[g_ss])
        h.TT(g_yb, g_yb[:], g_y, g_y[:], g_sg, g_sg[:], ALU.mult)
        put_out(g_yb, 1, j)


    def put_out(src, br, j):
        cnt["tr"] = cnt.get("tr", 0) + 1
        p = trslots[cnt["tr"] % len(trslots)]
        h.TR(p, p[:, :], src, src[:], cstb, identb)
        h.CP(yst[br], yst[br][:, j * 128:(j + 1) * 128], p, p[:, :])

    for st_i in range(NST):
        ut = UTt[st_i % 2]
        t0 = st_i * ST
        for k in range(0, 8, 4):
            S.dma("sp", ut[:, k:k + 4, :], UTv[:, k:k + 4, t0:t0 + ST], writes=[ut], partial=(k > 0))
        if "b" in en:
            fm_proj("gq", ut, g_qT, g_qT[:])
            fm_proj("gk", ut, g_kT, g_kT[:])
            fm_proj("gglr", ut, g_glr, g_glr[0:16, :])
        if "c" in en:
            ssd_prep(ut)
        if "a" in en:
            att_prep(ut, t0)
        for j in range(4):
            tm_proj(ut, j)
            if "a" in en:
                h.CP(a_V, a_V[:, st_i * 4 + j, 0:128], psT0, psT0[:, 0:128], eng="act")
            if "b" in en:
                gla_step(ut, j)
            if "c" in en:
                ssd_step(ut, j)
        if "d" in en:
            rw_prep(ut)
            for c in range(ST // 64):
                rw_chunk(c)
        if "a" in en:
            att_run(st_i)
        for br, e_ in enumerate(("a", "b", "c", "d")):
            if e_ in en:
                S.dma("sp", yT[br * 128:(br + 1) * 128, t0:t0 + ST], yst[br][:], reads=[yst[br]], writes=[yT], partial=True)
    S.finish_wait("sp")
    S.emit()
    return nc


import numpy as np
def bc(v, n=128):
    return np.ascontiguousarray(np.broadcast_to(np.asarray(v, np.float32)[None, :], (n, len(v))))
def mix_inputs(P, g, SL, lam_init):
    cols, off = core_cols(g)
    m = {"W": np.ascontiguousarray(P["w_in"][:, cols]), "consts": make_consts()}
    hs = slice(g * 64, (g + 1) * 64)
    m["gla_w2aug"] = np.ascontiguousarray(np.concatenate([P["gla_gate_w2"][:, hs], P["gla_gate_b"][None, hs]], 0))
    m["gla_norm_bc"] = bc(P["gla_norm"])

    gr = g // 2
    heads = [2 * g, 2 * g + 1, 2 * (g ^ 1), 2 * (g ^ 1) + 1]
    xcols = np.concatenate([g * 128 + np.arange(128), (g ^ 1) * 128 + np.arange(128)])
    ch = [xcols[:128], xcols[128:], 512 + gr * 128 + np.arange(128), 768 + gr * 128 + np.arange(128)]
    cw = np.zeros((128, 16), np.float32); cb = np.zeros((128, 4), np.float32)
    for gi, c in enumerate(ch):
        cw[:, gi * 4:(gi + 1) * 4] = P["ssd_conv_w"][:, c].T
        cb[:, gi] = P["ssd_conv_b"][c]
    m["ssd_convw"] = cw; m["ssd_convb"] = cb
    m["ssd_dtb_bc"] = bc(P["ssd_dt_bias"][heads]); m["ssd_alog_bc"] = bc(P["ssd_a_log"][heads]); m["ssd_d_bc"] = bc(P["ssd_d"][heads])
    m["ssd_norm_bc"] = bc(P["ssd_norm"][xcols])
    half = 32
    inv_freq = (10000.0 ** (-np.arange(half, dtype=np.float32) / half)).astype(np.float32)
    ang = np.arange(SL, dtype=np.float32)[None, :] * inv_freq[:, None]
    cos, sin = np.cos(ang).astype(np.float32), np.sin(ang).astype(np.float32)
    m["ropeC"] = np.ascontiguousarray(np.concatenate([cos, cos, cos, cos], 0))
    m["ropeS"] = np.ascontiguousarray(np.concatenate([-sin, sin, -sin, sin], 0))
    for i, k in enumerate(("da_lambda_q1", "da_lambda_k1", "da_lambda_q2", "da_lambda_k2")):
        m["da_l%d" % i] = bc(P[k])
    m["da_norm_bc"] = bc(P["da_norm"])
    m["lamc"] = bc(np.array([1.0 - lam_init, -lam_init], np.float32))
    RWS = [512, 512, 512, 64, 64, 128]
    offs = np.cumsum([0] + RWS)
    my = slice(g * 128, (g + 1) * 128)
    mu = np.zeros((128, 6), np.float32)
    mu[:, 0] = P["rw_mu"][offs[0]:offs[1]][my]; mu[:, 1] = P["rw_mu"][offs[1]:offs[2]][my]; mu[:, 2] = P["rw_mu"][offs[2]:offs[3]][my]
    mu[:64, 3] = P["rw_mu"][offs[3]:offs[4]]; mu[:64, 4] = P["rw_mu"][offs[4]:offs[5]]; mu[:, 5] = P["rw_mu"][offs[5]:offs[6]]
    m["rw_mu"] = mu
    m["rw_par"] = np.ascontiguousarray(np.stack([P["rw_w0"][my], P["rw_a0"][my], P["rw_k_k"][my], P["rw_k_a"][my], P["rw_r_k"][my]], 1))
    m["rw_nw_st"] = np.ascontiguousarray(P["rw_norm_w"][my].reshape(2, 1, 64).repeat(64, 1).reshape(128, 64))
    m["rw_nb_st"] = np.ascontiguousarray(P["rw_norm_b"][my].reshape(2, 1, 64).repeat(64, 1).reshape(128, 64))
    m["rw_w2"] = np.ascontiguousarray(P["rw_w2"][:, my]); m["rw_a2"] = np.ascontiguousarray(P["rw_a2"][:, my]); m["rw_g2"] = np.ascontiguousarray(P["rw_g2"][:, my])
    return m


SEQ = 8192
NCORE = 8


def _g8(v):
    return np.ascontiguousarray(np.asarray(v, np.float32).reshape(8, 128).T)


def _ffn_inputs(pfx, inp, which, l):
    return {pfx + "_g": _g8(inp[which + "_norm"][l]), pfx + "_wg": np.ascontiguousarray(inp[which + "_wg"][l]),
            pfx + "_wu": np.ascontiguousarray(inp[which + "_wu"][l]), pfx + "_wd": np.ascontiguousarray(inp[which + "_wd"][l])}


def kernel(**inp):
    inp = {k: np.asarray(v) for k, v in inp.items()}
    x = inp["x"].astype(np.float32)
    L = inp["w_in"].shape[0]
    cores = list(range(NCORE))
    hT = [np.ascontiguousarray(x[c // 4, (c % 4) * 2048:(c % 4 + 1) * 2048].T) for c in cores]
    P0 = build_tok(False, True, False)
    Pmid = build_tok(True, True, False)
    Plast = build_tok(True, False, True)
    Pmix = build_mix(SEQ, 0.0)
    maps = []
    for c in cores:
        m = {"hT_in": hT[c], "mixn_g": _g8(inp["mix_norm"][0])}
        m.update(_ffn_inputs("f1", inp, "ffn1", 0))
        maps.append(m)
    res = run_bass_kernel_spmd(P0, maps, core_ids=cores).results
    hT = [np.asarray(r["hT_out"]) for r in res]
    uT = [np.asarray(r["uT_out"]) for r in res]
    for l in range(L):
        lam_init = 0.8 - 0.6 * math.exp(-0.3 * l)
        Pl = {k: inp[k][l] for k in inp if k not in ("x", "final_norm")}
        UTb = [np.ascontiguousarray(np.concatenate([uT[b * 4 + j] for j in range(4)], axis=1)) for b in range(2)]
        maps = []
        for c in cores:
            m = mix_inputs(Pl, c % 4, SEQ, lam_init)
            m["UT"] = UTb[c // 4]
            maps.append(m)
        res = run_bass_kernel_spmd(Pmix, maps, core_ids=cores).results
        yT = [np.asarray(r["yT"]) for r in res]
        yfull = []
        for b in range(2):
            rows = []
            for br in range(4):
                for g in range(4):
                    rows.append(yT[b * 4 + g][br * 128:(br + 1) * 128])
            yfull.append(np.concatenate(rows, axis=0))
        gate_cols = np.ascontiguousarray(inp["w_in"][l][:, 6424:6424 + 4096])
        gb = np.ascontiguousarray(inp["gate_b"][l].reshape(4, 8, 128).transpose(2, 0, 1).reshape(128, 32))
        maps = []
        for c in cores:
            b, j = c // 4, c % 4
            m = {"hT_in": hT[c], "uT_in": uT[c], "yT_in": np.ascontiguousarray(yfull[b][:, j * 2048:(j + 1) * 2048]),
                 "w_gate": gate_cols, "w_branch": np.ascontiguousarray(inp["w_branch"][l].reshape(4 * 512, 1024)),
                 "w_out": np.ascontiguousarray(inp["w_out"][l]), "gate_b": gb}
            m.update(_ffn_inputs("f2", inp, "ffn2", l))
            if l + 1 < L:
                m.update(_ffn_inputs("f1", inp, "ffn1", l + 1))
                m["mixn_g"] = _g8(inp["mix_norm"][l + 1])
            else:
                m["final_g"] = _g8(inp["final_norm"])
            maps.append(m)
        res = run_bass_kernel_spmd(Pmid if l + 1 < L else Plast, maps, core_ids=cores).results
        hT = [np.asarray(r["hT_out"]) for r in res]
        if l + 1 < L:
            uT = [np.asarray(r["uT_out"]) for r in res]
    out = np.zeros((2, SEQ, 1024), np.float32)
    for c in cores:
        out[c // 4, (c % 4) * 2048:(c % 4 + 1) * 2048] = hT[c].T
    return out
```

```python
import math
import numpy as np
import ml_dtypes
from concourse.bass_utils import run_bass_kernel_spmd

import numpy as np
import concourse.bass as bass
import concourse.mybir as mybir
from contextlib import ExitStack

F32 = mybir.dt.float32
BF16 = mybir.dt.bfloat16
AF = mybir.ActivationFunctionType
ALU = mybir.AluOpType
AX = mybir.AxisListType

ENGS = ("pe", "act", "dve", "pool", "sp")


class Buf:
    __slots__ = ("name", "t", "last_w", "readers")

    def __init__(self, name, t):
        self.name = name
        self.t = t
        self.last_w = []
        self.readers = []

    def __getitem__(self, k):
        return self.t[k]


class SubBuf:
    def __init__(self, parent, ap, name=None):
        self.parent = parent
        self.t = ap
        self.name = name or parent.name

    @property
    def last_w(self):
        return self.parent.last_w

    @last_w.setter
    def last_w(self, v):
        self.parent.last_w = v

    @property
    def readers(self):
        return self.parent.readers

    @readers.setter
    def readers(self, v):
        self.parent.readers = v

    def __getitem__(self, k):
        return self.t[k]


class Sched:
    def __init__(self, nc, n_dma_sems=24):
        self.nc = nc
        self.es = ExitStack()
        self.q = {e: [] for e in ENGS}
        self.cnt = {e: 0 for e in ENGS}
        self.sem = {e: self.es.enter_context(nc.semaphore("s_" + e)) for e in ENGS if e != "sp"}
        self.seen = {e: {} for e in ENGS}
        self.dsem = [self.es.enter_context(nc.semaphore("d%d" % i)) for i in range(n_dma_sems)]
        self.dval = [0] * n_dma_sems
        self.dnext = 0
        self.dnext_pool = 0
        self.nbuf = 0

    def sb(self, shape, dt, name=None):
        self.nbuf += 1
        name = name or ("sb%d" % self.nbuf)
        t = self.es.enter_context(self.nc.sbuf_tensor(name, list(shape), dt))
        return Buf(name, t)

    def ps(self, shape, dt=F32, name=None):
        self.nbuf += 1
        name = name or ("ps%d" % self.nbuf)
        t = self.es.enter_context(self.nc.psum_tensor(name, list(shape), dt))
        return Buf(name, t)

    def dram(self, name, shape, dt, kind):
        t = self.nc.dram_tensor(name, list(shape), dt, kind=kind)
        return Buf(name, t.ap())

    def view(self, name, ap):
        return Buf(name, ap)

    def _wait(self, eng, dep):
        if dep is None:
            return
        if dep[0] == "dma":
            _, si, val = dep
            key = ("d", si)
            sem = self.dsem[si]
        else:
            e2, val = dep
            if e2 == "pe" and eng == "pe":
                return
            key = e2
            sem = self.sem[e2]
        if self.seen[eng].get(key, 0) >= val:
            return
        self.seen[eng][key] = val
        self.q[eng].append(("wait", sem, val))

    def _deps(self, eng, reads, writes, partial=False):
        for b in reads:
            for w in b.last_w:
                self._wait(eng, w)
        for b in writes:
            if not partial:
                for w in b.last_w:
                    self._wait(eng, w)
            for r in b.readers:
                self._wait(eng, r)

    def op(self, eng, fn, reads=(), writes=(), *args, **kw):
        if isinstance(fn, str):
            name = fn
            fn = (lambda e, name=name, args=args, kw=kw: getattr(e, name)(*args, **kw))
        self._deps(eng, reads, writes)
        self.cnt[eng] += 1
        idx = self.cnt[eng]
        self.q[eng].append(("op", fn, self.sem[eng]))
        me = (eng, idx)
        for b in reads:
            b.readers.append(me)
        for b in writes:
            b.last_w = [me]
            b.readers = []
        return me

    def dma(self, eng, out_ap, in_ap, reads=(), writes=(), partial=False, **kw):
        self._deps(eng, reads, writes, partial)
        half = len(self.dsem) // 2
        if eng == "pool":
            si = half + self.dnext_pool
            self.dnext_pool = (self.dnext_pool + 1) % half
        else:
            si = self.dnext
            self.dnext = (self.dnext + 1) % half
        if self.dval[si] > 0:
            self._wait(eng, ("dma", si, self.dval[si]))
        self.dval[si] += 16
        val = self.dval[si]
        sem = self.dsem[si]
        self.q[eng].append(("dma", out_ap, in_ap, sem, kw))
        me = ("dma", si, val)
        for b in reads:
            b.readers.append(me)
        for b in writes:
            if partial:
                b.last_w = b.last_w + [me]
            else:
                b.last_w = [me]
                b.readers = []
        return me

    def coll(self, kind, out_ap, in_ap, groups, reads=(), writes=()):
        eng = "pool"
        self._deps(eng, reads, writes)
        half = len(self.dsem) // 2
        si = half + self.dnext_pool
        self.dnext_pool = (self.dnext_pool + 1) % half
        if self.dval[si] > 0:
            self._wait(eng, ("dma", si, self.dval[si]))
        self.dval[si] += 16
        val = self.dval[si]
        self.q[eng].append(("coll", kind, out_ap, in_ap, groups, self.dsem[si]))
        me = ("dma", si, val)
        for b in reads:
            b.readers.append(me)
        for b in writes:
            b.last_w = [me]
            b.readers = []
        return me

    def barrier(self):
        for e in ENGS:
            for e2 in ENGS:
                if e2 != "sp" and self.cnt[e2] > 0:
                    self._wait(e, (e2, self.cnt[e2]))
            for si in range(len(self.dsem)):
                if self.dval[si] > 0:
                    self._wait(e, ("dma", si, self.dval[si]))

    def finish_wait(self, eng, bufs=None):
        for si in range(len(self.dsem)):
            if self.dval[si] > 0:
                self._wait(eng, ("dma", si, self.dval[si]))

    def emit(self):
        nc = self.nc
        q = self.q

        def run(engobj, lst):
            for it in lst:
                if it[0] == "wait":
                    engobj.wait_ge(it[1], it[2])
                elif it[0] == "op":
                    it[1](engobj).then_inc(it[2], 1)
                elif it[0] == "coll":
                    _, kind, o, i, groups, sem = it
                    engobj.collective_compute(kind, mybir.AluOpType.bypass, ins=[i], outs=[o],
                                              replica_groups=groups).then_inc(sem, 16)
                else:
                    _, o, i, sem, kw = it
                    engobj.dma_start(out=o, in_=i, **kw).then_inc(sem, 16)

        with nc.Block() as block:
            @block.tensor
            def _(e):
                run(e, q["pe"])

            @block.scalar
            def _(e):
                run(e, q["act"])

            @block.vector
            def _(e):
                run(e, q["dve"])

            @block.gpsimd
            def _(e):
                run(e, q["pool"])

            @block.sync
            def _(e):
                run(e, q["sp"])
        self.es.close()


D = 1024
TT = 512
FF = 2816
FG = 256
NFG = FF // FG
EPS = 1e-6
NBR = 4
MW = 512


def build_tok(do_merge, do_ffn1, do_final, NT=2048):
    NTT = NT // TT
    nc = bass.Bass("TRN2", target_bir_lowering=False)
    S = Sched(nc)
    hT_in = S.dram("hT_in", [D, NT], F32, "ExternalInput")
    hT_out = S.dram("hT_out", [D, NT], F32, "ExternalOutput")
    hT = S.sb([128, 8, NT], F32, "hT")
    arenaA = S.sb([128, max(8 * NT, 32 * TT)], BF16, "arenaA")
    WSZ = 8 * FG
    arenaW = S.sb([128, 6 * WSZ], BF16, "arenaW")
    ones = S.sb([128, 128], F32, "ones")
    epsb = S.sb([128, 1], F32, "epsb")
    sq = [S.sb([128, TT], F32, "sq%d" % i) for i in range(2)]
    rstd = S.sb([128, TT], F32, "rstd")
    tmpf = [S.sb([128, TT], F32, "tmpf%d" % i) for i in range(2)]
    abuf = [[S.sb([128, TT], BF16, "abuf%d_%d" % (i, c)) for c in range(2)] for i in range(2)]
    psA = [S.ps([128, TT], F32, "psA%d" % i) for i in range(4)]
    psO = [S.ps([128, TT], F32, "psO%d" % i) for i in range(2)]
    psN = S.ps([128, TT], F32, "psN")
    cnt = {"a": 0, "o": 0, "t": 0}

    def nxt(lst, key):
        b = lst[cnt[key] % len(lst)]
        cnt[key] += 1
        return b

    S.op("pool", "memset", [], [ones], ones[:], 1.0 / D)
    S.op("pool", "memset", [], [epsb], epsb[:], EPS)
    for k in range(8):
        S.dma("sp", hT[:, k, :], hT_in[k * 128:(k + 1) * 128, :], writes=[hT], partial=(k > 0))

    def load_small(name, shape):
        gd = S.dram(name, list(shape), F32, "ExternalInput")
        g = S.sb(list(shape), F32, name + "_sb")
        S.dma("sp", g[:], gd[:], writes=[g])
        return g

    unT = arenaA[:, 0:8 * NT].rearrange("p (k t) -> p k t", t=NT)

    def norm(g, out_f32=False):
        for t in range(NTT):
            ts = slice(t * TT, (t + 1) * TT)
            for k in range(8):
                s_ = sq[k % 2]
                S.op("act", "activation", [hT], [s_], out=s_[:], in_=hT[:, k, ts], func=AF.Square)
                S.op("pe", "matmul", [ones, s_], [psN], out=psN[:], lhsT=ones[:], rhs=s_[:], start=(k == 0), stop=(k == 7))
            S.op("act", "activation", [psN, epsb], [rstd], out=rstd[:], in_=psN[:], func=AF.Sqrt, bias=epsb[:], scale=1.0)
            S.op("dve", "reciprocal", [rstd], [rstd], out=rstd[:], in_=rstd[:])
            for k in range(8):
                if out_f32:
                    S.op("dve", "scalar_tensor_tensor", [hT, g, rstd], [hT], out=hT[:, k, ts], in0=hT[:, k, ts], scalar=g[:, k:k + 1],
                         in1=rstd[:], op0=ALU.mult, op1=ALU.mult)
                else:
                    S.op("dve", "scalar_tensor_tensor", [hT, g, rstd], [arenaA], out=unT[:, k, ts], in0=hT[:, k, ts], scalar=g[:, k:k + 1],
                         in1=rstd[:], op0=ALU.mult, op1=ALU.mult)

    def ffn_stage(pfx):
        g = load_small(pfx + "_g", [128, 8])
        wg = S.dram(pfx + "_wg", [D, FF], F32, "ExternalInput")
        wu = S.dram(pfx + "_wu", [D, FF], F32, "ExternalInput")
        wd = S.dram(pfx + "_wd", [FF, D], F32, "ExternalInput")
        norm(g)
        slots = []
        for i in range(2):
            base = i * 3 * WSZ
            wgb = arenaW[:, base:base + WSZ].rearrange("p (k f) -> p k f", f=FG)
            wub = arenaW[:, base + WSZ:base + 2 * WSZ].rearrange("p (k f) -> p k f", f=FG)
            wdb = arenaW[:, base + 2 * WSZ:base + 3 * WSZ].rearrange("p (c d) -> p c d", d=D)
            slots.append((S.view("wg%d" % i, wgb), S.view("wu%d" % i, wub), S.view("wd%d" % i, wdb)))
        wg_v = wg[:].rearrange("(k p) f -> p k f", p=128)
        wu_v = wu[:].rearrange("(k p) f -> p k f", p=128)
        wd_v = wd[:].rearrange("(c p) d -> p c d", p=128)
        for gi in range(NFG):
            bg, bu, bd = slots[gi % 2]
            for k in range(0, 8, 2):
                S.dma("pool", bg.t[:, k:k + 2, :], wg_v[:, k:k + 2, gi * FG:(gi + 1) * FG], writes=[bg], partial=(k > 0))
                S.dma("pool", bu.t[:, k:k + 2, :], wu_v[:, k:k + 2, gi * FG:(gi + 1) * FG], writes=[bu], partial=(k > 0))
            S.dma("pool", bd.t, wd_v[:, 2 * gi:2 * gi + 2, :], writes=[bd])
            for t in range(NTT):
                ts = slice(t * TT, (t + 1) * TT)
                ab = abuf[cnt["t"] % 2]
                cnt["t"] += 1
                for c in range(2):
                    pg = nxt(psA, "a")
                    pu = nxt(psA, "a")
                    for k in range(8):
                        S.op("pe", "matmul", [bg, arenaA], [pg], out=pg[:], lhsT=bg.t[:, k, c * 128:(c + 1) * 128], rhs=unT[:, k, ts],
                             start=(k == 0), stop=(k == 7))
                    for k in range(8):
                        S.op("pe", "matmul", [bu, arenaA], [pu], out=pu[:], lhsT=bu.t[:, k, c * 128:(c + 1) * 128], rhs=unT[:, k, ts],
                             start=(k == 0), stop=(k == 7))
                    tf = tmpf[c]
                    S.op("act", "activation", [pg], [tf], out=tf[:], in_=pg[:], func=AF.Silu)
                    S.op("dve", "tensor_tensor", [tf, pu], [ab[c]], out=ab[c][:], in0=tf[:], in1=pu[:], op=ALU.mult)
                for dc in range(8):
                    po = nxt(psO, "o")
                    for c in range(2):
                        S.op("pe", "matmul", [bd, ab[c]], [po], out=po[:], lhsT=bd.t[:, c, dc * 128:(dc + 1) * 128], rhs=ab[c][:],
                             start=(c == 0), stop=(c == 1))
                    S.op("dve", "scalar_tensor_tensor", [po, hT], [hT], out=hT[:, dc, ts], in0=po[:], scalar=0.5, in1=hT[:, dc, ts],
                         op0=ALU.mult, op1=ALU.add)

    def merge_stage():
        uT_in = S.dram("uT_in", [D, NT], BF16, "ExternalInput")
        yT_in = S.dram("yT_in", [NBR * MW, NT], BF16, "ExternalInput")
        wgate = S.dram("w_gate", [D, NBR * D], F32, "ExternalInput")
        wbr = S.dram("w_branch", [NBR * MW, D], F32, "ExternalInput")
        wout = S.dram("w_out", [D, D], F32, "ExternalInput")
        gb = load_small("gate_b", [128, NBR * 8])
        wo_sb = S.sb([128, 8, D], BF16, "wo_sb")
        wout_v = wout[:].rearrange("(k p) d -> p k d", p=128)
        for k in range(8):
            S.dma("pool", wo_sb[:, k, :], wout_v[:, k, :], writes=[wo_sb], partial=(k > 0))
        mrg = S.sb([128, 8, TT], F32, "mrg")
        yT_t = S.view("yT_t", arenaA[:, 0:16 * TT].rearrange("p (c t) -> p c t", t=TT))
        uT_t = S.view("uT_t", arenaA[:, 16 * TT:24 * TT].rearrange("p (k t) -> p k t", t=TT))
        mb = S.view("mb", arenaA[:, 24 * TT:32 * TT].rearrange("p (k t) -> p k t", t=TT))
        wgm = S.view("wgm", arenaW[:, 0:8 * D].rearrange("p (k d) -> p k d", d=D))
        wbm = S.view("wbm", arenaW[:, 8 * D:12 * D].rearrange("p (c d) -> p c d", d=D))
        wgate_v = wgate[:].rearrange("(k p) n -> p k n", p=128)
        wbr_v = wbr[:].rearrange("(c p) d -> p c d", p=128)
        yT_v = yT_in[:].rearrange("(c p) t -> p c t", p=128)
        uT_v = uT_in[:].rearrange("(k p) t -> p k t", p=128)
        for t in range(NTT):
            ts = slice(t * TT, (t + 1) * TT)
            for c in range(0, 16, 4):
                S.dma("sp", yT_t.t[:, c:c + 4, :], yT_v[:, c:c + 4, ts], writes=[yT_t], partial=(c > 0))
            for k in range(0, 8, 4):
                S.dma("sp", uT_t.t[:, k:k + 4, :], uT_v[:, k:k + 4, ts], writes=[uT_t], partial=(k > 0))
            for m in range(NBR):
                for k in range(8):
                    S.dma("pool", wgm.t[:, k, :], wgate_v[:, k, m * D:(m + 1) * D], writes=[wgm], partial=(k > 0))
                for c in range(4):
                    S.dma("pool", wbm.t[:, c, :], wbr_v[:, 4 * m + c, :], writes=[wbm], partial=(c > 0))
                for dc in range(8):
                    pg = nxt(psA, "a")
                    pp = nxt(psA, "a")
                    for k in range(8):
                        S.op("pe", "matmul", [wgm, uT_t], [pg], out=pg[:], lhsT=wgm.t[:, k, dc * 128:(dc + 1) * 128], rhs=uT_t.t[:, k, :],
                             start=(k == 0), stop=(k == 7))
                    for c in range(4):
                        S.op("pe", "matmul", [wbm, yT_t], [pp], out=pp[:], lhsT=wbm.t[:, c, dc * 128:(dc + 1) * 128], rhs=yT_t.t[:, 4 * m + c, :],
                             start=(c == 0), stop=(c == 3))
                    tf = tmpf[dc % 2]
                    S.op("act", "activation", [pg, gb], [tf], out=tf[:], in_=pg[:], func=AF.Sigmoid,
                         bias=gb[:, m * 8 + dc:m * 8 + dc + 1], scale=1.0)
                    if m == 0:
                        S.op("dve", "tensor_tensor", [tf, pp], [mrg], out=mrg[:, dc, :], in0=tf[:], in1=pp[:], op=ALU.mult)
                    else:
                        S.op("dve", "tensor_tensor", [tf, pp], [tf], out=tf[:], in0=tf[:], in1=pp[:], op=ALU.mult)
                        S.op("dve", "tensor_tensor", [tf, mrg], [mrg], out=mrg[:, dc, :], in0=mrg[:, dc, :], in1=tf[:], op=ALU.add)
            for k in range(8):
                S.op("act", "activation", [mrg], [mb], out=mb.t[:, k, :], in_=mrg[:, k, :], func=AF.Copy)
            for dc in range(8):
                po = nxt(psO, "o")
                for k in range(8):
                    S.op("pe", "matmul", [wo_sb, mb], [po], out=po[:], lhsT=wo_sb[:, k, dc * 128:(dc + 1) * 128], rhs=mb.t[:, k, :],
                         start=(k == 0), stop=(k == 7))
                S.op("dve", "tensor_tensor", [po, hT], [hT], out=hT[:, dc, ts], in0=hT[:, dc, ts], in1=po[:], op=ALU.add)

    if do_merge:
        merge_stage()
        S.barrier()
        ffn_stage("f2")
    if do_ffn1:
        if do_merge:
            S.barrier()
        ffn_stage("f1")
        uT_out = S.dram("uT_out", [D, NT], BF16, "ExternalOutput")
        g = load_small("mixn_g", [128, 8])
        S.barrier()
        norm(g)
        for k in range(8):
            S.dma("sp", uT_out[k * 128:(k + 1) * 128, :], unT[:, k, :], reads=[arenaA], writes=[uT_out], partial=True)
    if do_final:
        g = load_small("final_g", [128, 8])
        norm(g, out_f32=True)
    for k in range(8):
        S.dma("sp", hT_out[k * 128:(k + 1) * 128, :], hT[:, k, :], reads=[hT], writes=[hT_out], partial=True)
    S.finish_wait("sp")
    S.emit()
    return nc


D = 1024
ST = 512

DA0, GLA0, SSD0, RW0 = 0, 1536, 3088, 4632


def core_cols(g):
    import numpy as np
    gr = g // 2
    cols = []
    off = {}

    def add(name, idx):
        off[name] = (len(cols), len(idx))
        cols.extend(list(idx))

    def swap(idx):
        idx = np.asarray(idx)
        return np.concatenate([np.concatenate([idx[b + 32:b + 64], idx[b:b + 32]]) for b in range(0, len(idx), 64)])

    r = np.arange
    add("aq", DA0 + g * 128 + r(128))
    add("aqs", swap(DA0 + g * 128 + r(128)))
    add("ak", DA0 + 512 + g * 128 + r(128))
    add("aks", swap(DA0 + 512 + g * 128 + r(128)))
    add("gq", GLA0 + g * 64 + r(64))
    add("gk", GLA0 + 256 + g * 64 + r(64))
    add("gglr", GLA0 + 1536 + r(16))
    add("sx0", SSD0 + 512 + g * 128 + r(128))
    add("sx1", SSD0 + 512 + (g ^ 1) * 128 + r(128))
    add("sB", SSD0 + 512 + 512 + gr * 128 + r(128))
    add("sC", SSD0 + 512 + 768 + gr * 128 + r(128))
    add("rr", RW0 + g * 128 + r(128))
    add("rk", RW0 + 512 + g * 128 + r(128))
    add("rv", RW0 + 1024 + g * 128 + r(128))
    add("rwl", RW0 + 1536 + r(64))
    add("ral", RW0 + 1600 + r(64))
    add("rgl", RW0 + 1664 + r(128))
    add("T0", np.concatenate([DA0 + 1024 + g * 128 + r(128), GLA0 + 512 + g * 128 + r(128), GLA0 + 1024 + g * 128 + r(128),
                              GLA0 + 256 + g * 64 + r(64)]))
    add("T1", np.concatenate([SSD0 + g * 128 + r(128), SSD0 + (g ^ 1) * 128 + r(128),
                              SSD0 + 1536 + g * 2 + r(2), SSD0 + 1536 + (g ^ 1) * 2 + r(2)]))
    return np.asarray(cols), off


NCOL = len(core_cols(0)[0])
OFF = core_cols(0)[1]


class H:
    def __init__(self, S):
        self.S = S

    def MM(self, o, oap, l, lap, r, rap, st=True, sp=True):
        self.S.op("pe", "matmul", [l, r], [o], out=oap, lhsT=lap, rhs=rap, start=st, stop=sp, skip_group_check=True)

    def TR(self, o, oap, i, iap, ident, idap):
        self.S.op("pe", "transpose", [i, ident], [o], out=oap, in_=iap, identity=idap)

    def ACT(self, o, oap, i, iap, func, bias=None, scale=1.0, rd=(), wr=(), **kw):
        k = dict(out=oap, in_=iap, func=func, scale=scale)
        if bias is not None:
            k["bias"] = bias
        k.update(kw)
        self.S.op("act", "activation", [i] + list(rd), [o] + list(wr), **k)

    def TT(self, o, oap, a, aap, b, bap, op, eng="dve"):
        self.S.op(eng, "tensor_tensor", [a, b], [o], out=oap, in0=aap, in1=bap, op=op)

    def TS(self, o, oap, a, aap, s1, op0, s2=None, op1=None, rd=(), eng="dve"):
        k = dict(out=oap, in0=aap, scalar1=s1, scalar2=s2, op0=op0)
        if op1 is not None:
            k["op1"] = op1
        self.S.op(eng, "tensor_scalar", [a] + list(rd), [o], **k)

    def STT(self, o, oap, a, aap, sc, b, bap, op0, op1, rd=()):
        self.S.op("dve", "scalar_tensor_tensor", [a, b] + list(rd), [o], out=oap, in0=aap, scalar=sc, in1=bap, op0=op0, op1=op1)

    def CP(self, o, oap, i, iap, eng="dve"):
        if eng == "act":
            self.S.op("act", "activation", [i], [o], out=oap, in_=iap, func=AF.Copy)
        else:
            self.S.op(eng, "tensor_copy", [i], [o], out=oap, in_=iap)


C_ID, C_TRI, C_TRIG, C_NEGM, C_MASKA, C_I2, C_RWS, C_RWI, C_RWST, C_BD = range(10)
NCONST = 10


def make_consts():
    import numpy as np
    c = np.zeros((128, NCONST, 128), np.float32)
    j = np.arange(128)[:, None]
    i = np.arange(128)[None, :]
    c[:, C_ID] = (j == i)
    c[:, C_TRI] = (j <= i)
    c[:, C_TRIG] = (j <= i) * (-1.0 / 16.0)
    c[:, C_NEGM] = np.where(j > i, -30000.0, 0.0)
    c[:, C_MASKA] = ((j // 64) <= (i // 64))
    c[:, C_I2, :64] = (j % 64 == i[:, :64])
    same = (j // 64) == (i // 64)
    c[:, C_RWS] = same & ((j % 64) < (i % 64))
    c[:, C_RWI] = same & ((j % 64) <= (i % 64))
    c[:, C_RWST] = same & ((j % 64) > (i % 64))
    c[:, C_BD] = same
    return c.reshape(128, NCONST * 128)


def build_mix(S_len, lam_init, en=("a", "b", "c", "d")):
    nc = bass.Bass("TRN2", target_bir_lowering=False)
    S = Sched(nc)
    h = H(S)
    NST = S_len // ST
    NKT = S_len // 128
    UT = S.dram("UT", [D, S_len], BF16, "ExternalInput")
    Wd = S.dram("W", [D, NCOL], F32, "ExternalInput")
    consts_d = S.dram("consts", [128, NCONST * 128], F32, "ExternalInput")
    yT = S.dram("yT", [512, S_len], BF16, "ExternalOutput")

    W = S.sb([128, 8, NCOL], BF16, "Wsb")
    Wv = Wd[:].rearrange("(k p) n -> p k n", p=128)
    for k in range(8):
        S.dma("pool", W[:, k, :], Wv[:, k, :], writes=[W], partial=(k > 0))
    cst = S.sb([128, NCONST, 128], F32, "cst")
    S.dma("sp", cst[:].rearrange("p a b -> p (a b)"), consts_d[:], writes=[cst])
    cstb = S.sb([128, NCONST, 128], BF16, "cstb")
    S.op("dve", "tensor_copy", [cst], [cstb], out=cstb[:], in_=cst[:])
    ident = cst[:, C_ID, :]
    identb = cstb[:, C_ID, :]
    epsb = S.sb([128, 1], F32, "epsb")
    wtmp = S.sb([128, ST], F32, "wtmp")
    S.op("pool", "memset", [], [epsb], epsb[:], 1e-6)

    def small(name, shape):
        d_ = S.dram(name, list(shape), F32, "ExternalInput")
        t = S.sb(list(shape), F32, name + "_sb")
        S.dma("sp", t[:], d_[:], writes=[t])
        return t

    psF = [S.ps([128, ST], F32, "psF%d" % i) for i in range(2)]
    psW = [S.ps([128, 4, 128], F32, "psW%d" % i) for i in range(3)]
    psGS = S.ps([128, 4, 128], F32, "psGS")
    psR = S.ps([128, 4, 128], F32, "psR")
    psTR = S.ps([128, 8, 128], BF16, "psTR")
    trslots = [SubBuf(psTR, psTR[:, j, :]) for j in range(8)]
    slots_g = [SubBuf(psGS, psGS[:, j, :]) for j in (0, 1)]
    slots_s = [SubBuf(psGS, psGS[:, j, :]) for j in (2, 3)]
    slots_r = [SubBuf(psR, psR[:, j, :]) for j in range(4)]
    cnt = {"f": 0, "s": 0, "g": 0, "ss": 0, "r": 0}
    cur = {}

    def PSg():
        cnt["g"] += 1
        return slots_g[cnt["g"] % 2]

    def PSs():
        cnt["ss"] += 1
        return slots_s[cnt["ss"] % 2]

    def PSr():
        cnt["r"] += 1
        return slots_r[cnt["r"] % 4]

    def PF():
        cnt["f"] += 1
        return psF[cnt["f"] % 2]


    UTt = [S.sb([128, 8, ST], BF16, "UTt0")] * 2
    UTv = UT[:].rearrange("(k p) t -> p k t", p=128)

    def fm_proj(name, ut, dst, dst_ap, rows=None):
        o, n = OFF[name]
        p = PF()
        for k in range(8):
            h.MM(p, p[0:n, :], W, W[:, k, o:o + n], ut, ut[:, k, :], st=(k == 0), sp=(k == 7))
        h.CP(dst, dst_ap, p, p[0:n, :], eng="act")

    yst = [S.sb([128, ST], BF16, "yst%d" % i) for i in range(4)]

    if "b" in en:
        gW2 = small("gla_w2aug", [17, 64])
        gnb = small("gla_norm_bc", [128, 128])
        g_glr = S.sb([17, ST], F32, "g_glr")
        S.op("pool", "memset", [], [g_glr], g_glr[:], 1.0)
        g_qT = S.sb([64, ST], F32, "g_qT")
        g_kT = S.sb([64, ST], F32, "g_kT")
        g_S = S.sb([64, 128], F32, "g_S")
        g_Sb = S.sb([64, 128], BF16, "g_Sb")
        S.op("pool", "memset", [], [g_S], g_S[:], 0.0)
        S.op("pool", "memset", [], [g_Sb], g_Sb[:], 0.0)
        g_e1 = S.sb([128, 64], F32, "g_e1")
        g_l = S.sb([128, 64], F32, "g_l")
        g_eGT = S.sb([64, 128], F32, "g_eGT")
        g_enGT = S.sb([64, 128], F32, "g_enGT")
        g_enG = S.sb([128, 64], F32, "g_enG")
        g_dec = S.sb([64, 1], F32, "g_dec")
        g_qin = S.sb([64, 128], BF16, "g_qin")
        g_ktT = S.sb([64, 128], BF16, "g_ktT")
        g_kt = S.sb([128, 64], BF16, "g_kt")
        g_v = [S.sb([128, 128], BF16, "g_v%d" % i) for i in range(4)]
        g_kraw = [S.sb([128, 64], F32, "g_kraw%d" % i) for i in range(4)]
        g_att = S.sb([128, 128], BF16, "g_att")
        g_y = S.sb([128, 128], F32, "g_y")
        g_sq = S.sb([128, 128], F32, "g_sq")
        g_ss = S.sb([128, 1], F32, "g_ss")
        g_sg = [S.sb([128, 128], F32, "g_sg%d" % i) for i in range(4)]
        g_yb = S.sb([128, 128], BF16, "g_yb")


    if "c" in en:
        s_cw = small("ssd_convw", [128, 16])
        s_cb = small("ssd_convb", [128, 4])
        s_dtb = small("ssd_dtb_bc", [128, 4])
        s_alog = small("ssd_alog_bc", [128, 4])
        s_D = small("ssd_d_bc", [128, 4])
        s_nb = small("ssd_norm_bc", [128, 256])
        s_A = S.sb([128, 4], F32, "s_A")
        h.ACT(s_A, s_A[:], s_alog, s_alog[:], AF.Exp)
        h.TS(s_A, s_A[:], s_A, s_A[:], -1.0, ALU.mult)
        s_ones = S.sb([128, 128], F32, "s_ones")
        S.op("pool", "memset", [], [s_ones], s_ones[:], 1.0)
        s_hb = [S.sb([128, 3 + ST], F32, "s_hb%d" % i) for i in range(4)]
        for b_ in s_hb:
            S.op("pool", "memset", [], [b_], b_[:], 0.0)
        s_acc = wtmp
        s_fm = [S.sb([128, ST], BF16, "s_fm%d" % i) for i in range(4)]
        s_x = S.sb([128, 256], BF16, "s_x")
        s_B = S.sb([128, 128], BF16, "s_B")
        s_dt = S.sb([128, 4], F32, "s_dt")
        s_dtA = S.sb([128, 4], F32, "s_dtA")
        s_nac = S.sb([128, 4], F32, "s_nac")
        s_eac = S.sb([128, 4], F32, "s_eac")
        s_cd = S.sb([128, 4], F32, "s_cd")
        s_sc = S.sb([128, 128], F32, "s_sc")
        s_bc = S.sb([128, 128], F32, "s_bc")
        s_LT = S.sb([128, 128], F32, "s_LT")
        s_scm = S.sb([128, 128], BF16, "s_scm")
        s_xdt = S.sb([128, 256], BF16, "s_xdt")
        s_xdec = S.sb([128, 256], BF16, "s_xdec")
        s_yi = S.sb([128, 256], F32, "s_yi")
        s_y = S.sb([128, 256], F32, "s_y")
        s_sz = [S.sb([128, 256], F32, "s_sz%d" % i) for i in range(4)]
        s_dtr = [S.sb([128, 4], F32, "s_dtr%d" % i) for i in range(4)]
        s_sq = S.sb([128, 256], F32, "s_sq")
        s_ss = S.sb([128, 1], F32, "s_ss")
        s_yc = S.sb([128, 128], BF16, "s_yc")
        s_h = S.sb([128, 256], F32, "s_h")
        s_hbf = S.sb([128, 256], BF16, "s_hbf")
        S.op("pool", "memset", [], [s_h], s_h[:], 0.0)
        S.op("pool", "memset", [], [s_hbf], s_hbf[:], 0.0)

    def ssd_prep(ut):
        for gi, name in enumerate(("sx0", "sx1", "sB", "sC")):
            hb = s_hb[gi]
            h.CP(hb, hb[:, 0:3], hb, hb[:, ST:ST + 3], eng="pool")
            fm_proj(name, ut, hb, hb[:, 3:3 + ST])
            h.TS(s_acc, s_acc[:], hb, hb[:, 0:ST], s_cw[:, gi * 4:gi * 4 + 1], ALU.mult, rd=[s_cw])
            for k in range(1, 4):
                h.STT(s_acc, s_acc[:], hb, hb[:, k:k + ST], s_cw[:, gi * 4 + k:gi * 4 + k + 1], s_acc, s_acc[:], ALU.mult, ALU.add, rd=[s_cw])
            h.ACT(s_fm[gi], s_fm[gi][:], s_acc, s_acc[:], AF.Silu, bias=s_cb[:, gi:gi + 1], rd=[s_cb])

    def tr_bf(dst, dst_ap, src, src_ap):
        cnt["tr"] = cnt.get("tr", 0) + 1
        p = trslots[1 + cnt["tr"] % 2]
        h.TR(p, p[:, :], src, src_ap, cstb, identb)
        h.CP(dst, dst_ap, p, p[:, :])

    def ssd_step(ut, j):
        js = slice(j * 128, (j + 1) * 128)
        yield
        TRI = cst[:, C_TRI, :]
        tr_bf(s_x, s_x[:, 0:128], s_fm[0], s_fm[0][:, js])
        yield
        tr_bf(s_x, s_x[:, 128:256], s_fm[1], s_fm[1][:, js])
        yield
        tr_bf(s_B, s_B[:], s_fm[2], s_fm[2][:, js])
        yield
        h.ACT(s_dt, s_dt[:], s_dtr[j], s_dtr[j][:], AF.Exp)
        yield
        h.ACT(s_dt, s_dt[:], s_dt, s_dt[:], AF.Ln, bias=1.0)
        yield
        h.TT(s_dtA, s_dtA[:], s_dt, s_dt[:], s_A, s_A[:], ALU.mult)
        yield
        pa = PSs()
        yield
        h.MM(pa, pa[:, 0:4], cst, TRI, s_dtA, s_dtA[:])
        yield
        h.TS(s_nac, s_nac[:], pa, pa[:, 0:4], -1.0, ALU.mult)
        yield
        h.ACT(s_eac, s_eac[:], pa, pa[:, 0:4], AF.Exp)
        yield
        for hp in range(2):
            pyi = PSs()
            yield
            h.MM(pyi, pyi[:, :], s_fm[3], s_fm[3][:, js], s_hbf, s_hbf[:, hp * 128:(hp + 1) * 128])
            yield
            for q_ in range(2):
                hh = hp * 2 + q_
                h.ACT(s_yi, s_yi[:, hh * 64:(hh + 1) * 64], pyi, pyi[:, q_ * 64:(q_ + 1) * 64], AF.Copy, scale=s_eac[:, hh:hh + 1], rd=[s_eac])
                yield
        psc = PSs()
        yield
        h.MM(psc, psc[:, :], s_fm[2], s_fm[2][:, js], s_fm[3], s_fm[3][:, js])
        yield
        h.CP(s_sc, s_sc[:], psc, psc[:, :], eng="act")
        yield
        for hh in range(4):
            hb_ = slice(hh * 64, (hh + 1) * 64)
            yield
            h.TS(s_bc, s_bc[:], s_ones, s_ones[:], s_dtA[:, hh:hh + 1], ALU.mult, rd=[s_dtA])
            yield
            pr = PSs()
            yield
            h.MM(pr, pr[:, :], s_bc, s_bc[:], cst, TRI, st=True, sp=False)
            h.MM(pr, pr[:, :], cst, ident, cst, cst[:, C_NEGM, :], st=False, sp=True)
            yield
            h.ACT(s_LT, s_LT[:], pr, pr[:, :], AF.Exp, bias=s_nac[:, hh:hh + 1], rd=[s_nac])
            yield
            h.ACT(s_cd, s_cd[:, hh:hh + 1], pr, pr[:, 127:128], AF.Exp)
            yield
            h.TT(s_scm, s_scm[:], s_sc, s_sc[:], s_LT, s_LT[:], ALU.mult)
            yield
            h.TS(s_xdt, s_xdt[:, hb_], s_x, s_x[:, hb_], s_dt[:, hh:hh + 1], ALU.mult, rd=[s_dt])
            yield
            py = PSs()
            yield
            h.MM(py, py[:, 0:64], s_scm, s_scm[:], s_xdt, s_xdt[:, hb_])
            yield
            h.TT(s_y, s_y[:, hb_], s_yi, s_yi[:, hb_], py, py[:, 0:64], ALU.add)
            yield
            h.STT(s_y, s_y[:, hb_], s_x, s_x[:, hb_], s_D[:, hh:hh + 1], s_y, s_y[:, hb_], ALU.mult, ALU.add, rd=[s_D])
            yield
            h.TS(s_xdec, s_xdec[:, hb_], s_xdt, s_xdt[:, hb_], s_LT[:, 127:128], ALU.mult, rd=[s_LT])
            yield
        for hp in range(2):
            pcs = PSs()
            yield
            h.MM(pcs, pcs[:, :], s_B, s_B[:], s_xdec, s_xdec[:, hp * 128:(hp + 1) * 128])
            yield
            for q_ in range(2):
                hh = hp * 2 + q_
                hb_ = slice(hh * 64, (hh + 1) * 64)
                yield
                h.STT(s_h, s_h[:, hb_], s_h, s_h[:, hb_], s_cd[:, hh:hh + 1], pcs, pcs[:, q_ * 64:(q_ + 1) * 64], ALU.mult, ALU.add, rd=[s_cd])
                yield
        h.CP(s_hbf, s_hbf[:], s_h, s_h[:], eng="act")
        yield
        h.TT(s_y, s_y[:], s_y, s_y[:], s_sz[j], s_sz[j][:], ALU.mult)
        yield
        h.ACT(s_sq, s_sq[:], s_y, s_y[:], AF.Square, accum_out=s_ss[:], scale=(1.0 / 256.0) ** 0.5, wr=[s_ss])
        yield
        h.ACT(s_ss, s_ss[:], s_ss, s_ss[:], AF.Sqrt, bias=epsb[:], rd=[epsb])
        yield
        S.op("dve", "reciprocal", [s_ss], [s_ss], out=s_ss[:], in_=s_ss[:])
        yield
        h.STT(s_yc, s_yc[:], s_y, s_y[:, 0:128], s_ss[:, 0:1], s_nb, s_nb[:, 0:128], ALU.mult, ALU.mult, rd=[s_ss])
        yield
        put_out(s_yc, 2, j)
        yield

    if "a" in en:
        ropeC = S.dram("ropeC", [128, S_len], F32, "ExternalInput")
        ropeS = S.dram("ropeS", [128, S_len], F32, "ExternalInput")
        a_lq = [small("da_l%d" % i, [128, 64]) for i in range(4)]
        a_nb = small("da_norm_bc", [128, 128])
        lamc = small("lamc", [128, 2])
        h.TS(a_nb, a_nb[:], a_nb, a_nb[:], lamc[:, 0:1], ALU.mult, rd=[lamc])
        a_t64 = S.sb([128, 64], F32, "a_t64")
        a_l1 = S.sb([128, 1], F32, "a_l1")
        a_l2 = S.sb([128, 1], F32, "a_l2")
        a_nlam = S.sb([128, 1], F32, "a_nlam")
        h.TT(a_t64, a_t64[:], a_lq[0], a_lq[0][:], a_lq[1], a_lq[1][:], ALU.mult)
        S.op("dve", "reduce_sum", [a_t64], [a_l1], out=a_l1[:], in_=a_t64[:], axis=AX.X)
        h.TT(a_t64, a_t64[:], a_lq[2], a_lq[2][:], a_lq[3], a_lq[3][:], ALU.mult)
        S.op("dve", "reduce_sum", [a_t64], [a_l2], out=a_l2[:], in_=a_t64[:], axis=AX.X)
        h.ACT(a_l1, a_l1[:], a_l1, a_l1[:], AF.Exp)
        h.ACT(a_l2, a_l2[:], a_l2, a_l2[:], AF.Exp)
        h.TT(a_nlam, a_nlam[:], a_l2, a_l2[:], a_l1, a_l1[:], ALU.subtract)
        h.TS(a_nlam, a_nlam[:], a_nlam, a_nlam[:], lamc[:, 1:2], ALU.add, rd=[lamc])
        a_cos = S.sb([128, ST], F32, "a_cos")
        a_sin = S.sb([128, ST], F32, "a_sin")
        a_f = [wtmp, S.sb([128, ST], F32, "a_f1")]
        a_qT = S.sb([128, ST], BF16, "a_qT")
        a_kT = S.sb([128, S_len], BF16, "a_kT")
        a_V = S.sb([128, NKT, 130], BF16, "a_V")
        S.op("pool", "memset", [], [a_V], a_V[:], 1.0)
        a_pT = [S.sb([128, ST], BF16, "a_pT%d" % i) for i in range(2)] * 2
        a_z = S.sb([1, 512], BF16, "a_z")
        S.op("pool", "memset", [], [a_z], a_z[:], 0.0)
        a_o = S.sb([128, 128], F32, "a_o")
        a_r = S.sb([128, 2], F32, "a_r")
        a_sq = S.sb([128, 128], F32, "a_sq")
        a_ss = S.sb([128, 1], F32, "a_ss")
        a_y = S.sb([128, 128], BF16, "a_y")
        accflat = [b_[:].rearrange("p a b -> p (a b)") for b_ in psW]

    def att_prep(ut, t0):
        S.dma("sp", a_cos[:], ropeC[:, t0:t0 + ST], writes=[a_cos])
        S.dma("sp", a_sin[:], ropeS[:, t0:t0 + ST], writes=[a_sin])
        for (n0, n1, dst, dap) in (("aq", "aqs", a_qT, a_qT[:]), ("ak", "aks", a_kT, a_kT[:, t0:t0 + ST])):
            x0, x1 = a_f[0], a_f[1]
            fm_proj(n0, ut, x0, x0[:])
            fm_proj(n1, ut, x1, x1[:])
            h.TT(x0, x0[:], x0, x0[:], a_cos, a_cos[:], ALU.mult)
            h.TT(x1, x1[:], x1, x1[:], a_sin, a_sin[:], ALU.mult)
            h.TT(dst, dap, x0, x0[:], x1, x1[:], ALU.add)

    def att_run(st_i):
        for b_i in range(3):
            h.MM(psW[b_i], accflat[b_i][:, 0:390], a_z, a_z[0:1, 0:128], a_z, a_z[0:1, 0:390], st=True, sp=True)
            yield
        nkt = 4 * st_i + 4
        ci = 0
        for kt in range(nkt):
            ktl = kt - 4 * st_i
            while ktl >= 0 and ktl not in cur["tm_done"]:
                yield "blocked"
            qb0 = max(0, ktl)
            yield
            nq0 = qb0 * 128
            for c in range(2):
                sT = PF()
                yield
                cs = slice(c * 64, (c + 1) * 64)
                yield
                h.MM(sT, sT[:, nq0:ST], a_kT, a_kT[cs, kt * 128:(kt + 1) * 128], a_qT, a_qT[cs, nq0:ST])
                pT = a_pT[ci % 4]
                ci += 1
                h.ACT(pT, pT[:, nq0:ST], sT, sT[:, nq0:ST], AF.Exp, scale=0.125)
                yield
                if ktl >= 0:
                    h.TT(pT, pT[:, nq0:nq0 + 128], pT, pT[:, nq0:nq0 + 128], cstb, cstb[:, C_MASKA, :], ALU.mult)
                    yield
                for qb in range(qb0, 4):
                    idx = qb * 2 + c
                    bk, of = idx // 3, (idx % 3) * 130
                    h.MM(psW[bk], accflat[bk][:, of:of + 130], pT, pT[:, qb * 128:(qb + 1) * 128], a_V, a_V[:, kt, :], st=False,
                         sp=(kt == 4 * st_i + qb))
                    yield
        for qb in range(4):
            i1, i2 = qb * 2, qb * 2 + 1
            b1, o1 = i1 // 3, (i1 % 3) * 130
            b2, o2 = i2 // 3, (i2 % 3) * 130
            S.op("dve", "reciprocal", [psW[b1]], [a_r], out=a_r[:, 0:1], in_=accflat[b1][:, o1 + 128:o1 + 129])
            yield
            S.op("dve", "reciprocal", [psW[b2], a_r], [a_r], out=a_r[:, 1:2], in_=accflat[b2][:, o2 + 128:o2 + 129])
            yield
            h.TT(a_r, a_r[:, 1:2], a_r, a_r[:, 1:2], a_nlam, a_nlam[:], ALU.mult)
            yield
            h.TS(a_o, a_o[:], psW[b1], accflat[b1][:, o1:o1 + 128], a_r[:, 0:1], ALU.mult, rd=[a_r])
            yield
            h.STT(a_o, a_o[:], psW[b2], accflat[b2][:, o2:o2 + 128], a_r[:, 1:2], a_o, a_o[:], ALU.mult, ALU.add, rd=[a_r])
            yield
            h.ACT(a_sq, a_sq[:], a_o, a_o[:], AF.Square, accum_out=a_ss[:], scale=(1.0 / 128.0) ** 0.5, wr=[a_ss])
            yield
            h.ACT(a_ss, a_ss[:], a_ss, a_ss[:], AF.Sqrt, bias=epsb[:], rd=[epsb])
            yield
            S.op("dve", "reciprocal", [a_ss], [a_ss], out=a_ss[:], in_=a_ss[:])
            yield
            h.STT(a_y, a_y[:], a_o, a_o[:], a_ss[:, 0:1], a_nb, a_nb[:], ALU.mult, ALU.mult, rd=[a_ss])
            yield
            put_out(a_y, 0, qb)
            yield

    if "d" in en:
        r_mu = small("rw_mu", [128, 6])
        r_par = small("rw_par", [128, 5])
        r_nw = small("rw_nw_st", [128, 64])
        r_nb = small("rw_nb_st", [128, 64])
        r_w2d = S.dram("rw_w2", [64, 128], F32, "ExternalInput")
        r_a2d = S.dram("rw_a2", [64, 128], F32, "ExternalInput")
        r_g2d = S.dram("rw_g2", [128, 128], F32, "ExternalInput")
        r_w2 = S.sb([64, 128], BF16, "r_w2"); S.dma("pool", r_w2[:], r_w2d[:], writes=[r_w2])
        r_a2 = S.sb([64, 128], BF16, "r_a2"); S.dma("pool", r_a2[:], r_a2d[:], writes=[r_a2])
        r_g2 = S.sb([128, 128], BF16, "r_g2"); S.dma("pool", r_g2[:], r_g2d[:], writes=[r_g2])
        r_eps2 = S.sb([128, 1], F32, "r_eps2")
        S.op("pool", "memset", [], [r_eps2], r_eps2[:], 64e-5)
        r_ones = S.sb([128, 64], F32, "r_ones")
        S.op("pool", "memset", [], [r_ones], r_ones[:], 1.0)
        RN = ("rr", "rk", "rv", "rwl", "ral", "rgl")
        RROWS = (128, 128, 128, 64, 64, 128)
        r_pb = [S.sb([128, 1 + ST], F32, "r_pb%d" % i) for i in range(6)]
        for b_ in r_pb:
            S.op("pool", "memset", [], [b_], b_[:], 0.0)
        r_mx = [S.sb([128, ST], F32, "r_mx%d" % i) for i in range(6)]
        r_tb = S.sb([128, ST], BF16, "r_tb")
        r_lw = S.sb([128, ST], F32, "r_lw")
        r_a = S.sb([128, ST], F32, "r_a")
        r_sgT = S.sb([128, ST], BF16, "r_sgT")
        r_kk = S.sb([128, ST], F32, "r_kk")
        r_t1 = wtmp
        r_cl = S.sb([128, ST], F32, "r_cl")
        r_Ep = S.sb([128, ST], F32, "r_Ep")
        r_Em = S.sb([128, ST], F32, "r_Em")
        r_Epr = r_t1
        r_At = r_cl
        r_Bt = r_lw
        r_Kt = r_mx[1]
        r_Rt = r_mx[0]
        r_RK = S.sb([128, ST], F32, "r_RK")
        r_bAR = [S.sb([128, 256], F32, "r_bAR%d" % i) for i in range(2)]
        r_bB = [S.sb([128, 128], F32, "r_bB%d" % i) for i in range(2)]
        r_bK = [S.sb([128, 128], F32, "r_bK%d" % i) for i in range(2)]
        r_bRK = [S.sb([128, 128], F32, "r_bRK%d" % i) for i in range(2)]
        r_bV = [S.sb([128, 128], F32, "r_bV%d" % i) for i in range(2)]
        r_Yb = S.sb([128, 128], F32, "r_Yb")
        for b_ in r_bAR + r_bB + r_bK + r_bRK + r_bV + [r_Yb]:
            S.op("pool", "memset", [], [b_], b_[:], 0.0)
        r_X = [S.sb([128, 128], F32, "r_X%d" % i) for i in range(2)]
        r_Y = [S.sb([128, 128], F32, "r_Y%d" % i) for i in range(2)]
        r_P = [S.sb([128, 128], F32, "r_P%d" % i) for i in range(2)]
        r_RBT = S.sb([128, 128], F32, "r_RBT")
        r_MkT = S.sb([128, 128], F32, "r_MkT")
        r_RKT = S.sb([128, 128], F32, "r_RKT")
        r_Btok = S.sb([128, 128], F32, "r_Btok")
        r_Ktok = S.sb([128, 128], F32, "r_Ktok")
        r_sV = S.sb([128, 64], F32, "r_sV")
        r_sW = S.sb([128, 64], F32, "r_sW")
        r_sU = S.sb([128, 64], F32, "r_sU")
        r_ST = S.sb([128, 64], F32, "r_ST")
        S.op("pool", "memset", [], [r_ST], r_ST[:], 0.0)
        r_bs = S.sb([128, 2], F32, "r_bs")
        r_y = S.sb([128, 64], F32, "r_y")
        r_j = S.sb([128, 64], F32, "r_j")
        r_m = S.sb([128, 1], F32, "r_m")
        r_v = S.sb([128, 1], F32, "r_v")

    def rw_prep(ut):
        for gi, name in enumerate(RN):
            n = RROWS[gi]
            pb = r_pb[gi]
            h.CP(pb, pb[0:n, 0:1], pb, pb[0:n, ST:ST + 1], eng="pool")
            fm_proj(name, ut, pb, pb[0:n, 1:1 + ST])
            mx = r_mx[gi]
            h.TT(mx, mx[0:n, :], pb, pb[0:n, 0:ST], pb, pb[0:n, 1:1 + ST], ALU.subtract)
            h.STT(mx, mx[0:n, :], mx, mx[0:n, :], r_mu[0:n, gi:gi + 1], pb, pb[0:n, 1:1 + ST], ALU.mult, ALU.add, rd=[r_mu])
        rr, rk, rv, rwl, ral, rgl = r_mx
        h.ACT(r_tb, r_tb[0:64, :], rwl, rwl[0:64, :], AF.Tanh)
        p = PF()
        h.MM(p, p[:, :], r_w2, r_w2[:], r_tb, r_tb[0:64, :])
        h.ACT(r_lw, r_lw[:], p, p[:, :], AF.Sigmoid, bias=r_par[:, 0:1], rd=[r_par])
        h.TS(r_lw, r_lw[:], r_lw, r_lw[:], -0.606531, ALU.mult)
        h.CP(r_tb, r_tb[0:64, :], ral, ral[0:64, :], eng="act")
        p = PF()
        h.MM(p, p[:, :], r_a2, r_a2[:], r_tb, r_tb[0:64, :])
        h.ACT(r_a, r_a[:], p, p[:, :], AF.Sigmoid, bias=r_par[:, 1:2], rd=[r_par])
        h.ACT(r_sgT, r_sgT[:], rgl, rgl[:], AF.Sigmoid)
        h.TS(r_kk, r_kk[:], rk, rk[:], r_par[:, 2:3], ALU.mult, rd=[r_par])
        h.TT(r_t1, r_t1[:], r_kk, r_kk[:], r_kk, r_kk[:], ALU.mult)
        p = PF()
        h.MM(p, p[:, :], cst, cst[:, C_BD, :], r_t1, r_t1[:])
        h.ACT(r_t1, r_t1[:], p, p[:, :], AF.Sqrt)
        h.TS(r_t1, r_t1[:], r_t1, r_t1[:], 1e-12, ALU.max)
        S.op("dve", "reciprocal", [r_t1], [r_t1], out=r_t1[:], in_=r_t1[:])
        h.TT(r_kk, r_kk[:], r_kk, r_kk[:], r_t1, r_t1[:], ALU.mult)
        h.TS(r_t1, r_t1[:], r_a, r_a[:], -1.0, ALU.add, r_par[:, 3:4], ALU.mult, rd=[r_par])
        h.TS(r_t1, r_t1[:], r_t1, r_t1[:], 1.0, ALU.add)
        h.TT(rk, rk[:], rk, rk[:], r_t1, r_t1[:], ALU.mult)
        for c in range(ST // 64):
            cs = slice(c * 64, (c + 1) * 64)
            S.op("dve", "tensor_tensor_scan", [r_ones, r_lw], [r_cl], out=r_cl[:, cs], data0=r_ones[:, 0:64], data1=r_lw[:, cs],
                 initial=0.0, op0=ALU.mult, op1=ALU.add)
        h.ACT(r_Ep, r_Ep[:], r_cl, r_cl[:], AF.Exp)
        h.ACT(r_Em, r_Em[:], r_cl, r_cl[:], AF.Exp, scale=-1.0)
        h.TT(r_t1, r_t1[:], r_cl, r_cl[:], r_lw, r_lw[:], ALU.subtract)
        h.ACT(r_Epr, r_Epr[:], r_t1, r_t1[:], AF.Exp)
        h.STT(r_RK, r_RK[:], rr, rr[:], r_par[:, 4:5], rk, rk[:], ALU.mult, ALU.mult, rd=[r_par])
        h.STT(r_At, r_At[:], r_kk, r_kk[:], -1.0, r_Epr, r_Epr[:], ALU.mult, ALU.mult)
        h.TT(r_Bt, r_Bt[:], r_kk, r_kk[:], r_a, r_a[:], ALU.mult)
        h.TT(r_Bt, r_Bt[:], r_Bt, r_Bt[:], r_Em, r_Em[:], ALU.mult)
        h.TT(r_Kt, r_Kt[:], rk, rk[:], r_Em, r_Em[:], ALU.mult)
        h.TT(r_Rt, r_Rt[:], rr, rr[:], r_Ep, r_Ep[:], ALU.mult)

    def rw_chunk(c):
        cs = slice(c * 64, (c + 1) * 64)
        yield
        par = c % 2
        bAR, bB, bK, bRK, bV = r_bAR[par], r_bB[par], r_bK[par], r_bRK[par], r_bV[par]
        rv = r_mx[2]
        for hh in range(2):
            ps_ = slice(hh * 64, (hh + 1) * 64)
            yield
            h.CP(bAR, bAR[ps_, hh * 64:(hh + 1) * 64], r_At, r_At[ps_, cs], eng="pool")
            yield
            h.CP(bAR, bAR[ps_, 128 + hh * 64:128 + (hh + 1) * 64], r_Rt, r_Rt[ps_, cs], eng="pool")
            yield
            h.CP(bB, bB[ps_, ps_], r_Bt, r_Bt[ps_, cs], eng="pool")
            yield
            h.CP(bK, bK[ps_, ps_], r_Kt, r_Kt[ps_, cs], eng="pool")
            yield
            h.CP(bRK, bRK[ps_, ps_], r_RK, r_RK[ps_, cs], eng="pool")
            yield
            h.CP(bV, bV[ps_, ps_], rv, rv[ps_, cs], eng="pool")
            yield
        bA = bAR[:, 0:128]
        bR = bAR[:, 128:256]
        p = PSr(); h.MM(p, p[:, :], bB, bB[:], bAR, bA)
        yield
        h.TT(r_Y[0], r_Y[0][:], p, p[:, :], cst, cst[:, C_RWS, :], ALU.mult)
        yield
        p = PSr(); h.MM(p, p[:, :], bB, bB[:], bAR, bR)
        yield
        h.TT(r_RBT, r_RBT[:], p, p[:, :], cst, cst[:, C_RWI, :], ALU.mult)
        yield
        p = PSr(); h.MM(p, p[:, :], bK, bK[:], bAR, bA)
        yield
        h.TT(r_MkT, r_MkT[:], p, p[:, :], cst, cst[:, C_RWS, :], ALU.mult)
        yield
        p = PSr(); h.MM(p, p[:, :], bK, bK[:], bAR, bR)
        yield
        h.TT(r_RKT, r_RKT[:], p, p[:, :], cst, cst[:, C_RWI, :], ALU.mult)
        yield
        p = PSr(); h.MM(p, p[:, :], bAR, bA, bB, bB[:])
        yield
        h.TT(r_X[0], r_X[0][:], p, p[:, :], cst, cst[:, C_RWST, :], ALU.mult)
        yield
        h.TT(r_P[0], r_P[0][:], r_Y[0], r_Y[0][:], cst, ident, ALU.add)
        yield
        xi, pi = 0, 0
        for lvl in range(5):
            Xc, Yc, Xn, Yn = r_X[xi], r_Y[xi], r_X[1 - xi], r_Y[1 - xi]
            p = PSr(); h.MM(p, p[:, :], Yc, Yc[:], Xc, Xc[:])
            yield
            h.CP(Xn, Xn[:], p, p[:, :], eng="act")
            yield
            if lvl < 4:
                p = PSr(); h.MM(p, p[:, :], Xc, Xc[:], Yc, Yc[:])
                yield
                h.CP(Yn, Yn[:], p, p[:, :], eng="act")
                yield
            Pc, Pn = r_P[pi], r_P[1 - pi]
            p = PSr(); h.MM(p, p[:, :], Xn, Xn[:], Pc, Pc[:])
            yield
            h.TT(Pn, Pn[:], Pc, Pc[:], p, p[:, :], ALU.add)
            yield
            xi, pi = 1 - xi, 1 - pi
        TT_ = r_P[pi]
        p = PSr(); h.TR(p, p[:, :], bB, bB[:], cst, ident)
        yield
        h.CP(r_Btok, r_Btok[:], p, p[:, :], eng="act")
        yield
        p = PSr(); h.TR(p, p[:, :], bK, bK[:], cst, ident)
        yield
        h.CP(r_Ktok, r_Ktok[:], p, p[:, :], eng="act")
        yield
        p = PSr(); h.MM(p, p[:, 0:64], bV, bV[:], cst, cst[:, C_I2, 0:64])
        yield
        h.CP(r_sV, r_sV[:], p, p[:, 0:64], eng="act")
        yield
        p = PSr(); h.MM(p, p[:, 0:2], bRK, bRK[:], r_ones, r_ones[:, 0:2])
        yield
        h.CP(r_bs, r_bs[:], p, p[:, 0:2])
        yield
        p = PSr()
        yield
        h.MM(p, p[:, 0:64], r_MkT, r_MkT[:], r_sV, r_sV[:], st=True, sp=False)
        h.MM(p, p[:, 0:64], bAR, bA, r_ST, r_ST[:], st=False, sp=True)
        yield
        h.CP(r_sW, r_sW[:], p, p[:, 0:64])
        yield
        p = PSr()
        yield
        h.MM(p, p[:, 0:64], TT_, TT_[:], r_sW, r_sW[:])
        yield
        h.CP(r_sU, r_sU[:], p, p[:, 0:64])
        yield
        pY = PSr()
        yield
        h.MM(pY, pY[:, 0:64], bAR, bR, r_ST, r_ST[:], st=True, sp=False)
        h.MM(pY, pY[:, 0:64], r_RBT, r_RBT[:], r_sU, r_sU[:], st=False, sp=False)
        h.MM(pY, pY[:, 0:64], r_RKT, r_RKT[:], r_sV, r_sV[:], st=False, sp=True)
        yield
        h.CP(r_y, r_y[:], pY, pY[:, 0:64], eng="act")
        yield
        p = PSr()
        yield
        h.MM(p, p[:, 0:64], r_Btok, r_Btok[:], r_sU, r_sU[:], st=True, sp=False)
        h.MM(p, p[:, 0:64], r_Ktok, r_Ktok[:], r_sV, r_sV[:], st=False, sp=True)
        yield
        h.TT(r_ST, r_ST[:], r_ST, r_ST[:], p, p[:, 0:64], ALU.add)
        yield
        h.TS(r_ST, r_ST[:], r_ST, r_ST[:], r_Ep[:, c * 64 + 63:c * 64 + 64], ALU.mult, rd=[r_Ep])
        yield
        h.ACT(r_j, r_j[:], r_y, r_y[:], AF.Copy, accum_out=r_m[:], scale=1.0 / 64.0, wr=[r_m])
        yield
        h.TS(r_y, r_y[:], r_y, r_y[:], r_m[:, 0:1], ALU.subtract, rd=[r_m])
        yield
        h.ACT(r_j, r_j[:], r_y, r_y[:], AF.Square, accum_out=r_v[:], scale=0.125, wr=[r_v])
        yield
        h.ACT(r_v, r_v[:], r_v, r_v[:], AF.Sqrt, bias=r_eps2[:], rd=[r_eps2])
        yield
        S.op("dve", "reciprocal", [r_v], [r_v], out=r_v[:], in_=r_v[:])
        yield
        h.STT(r_y, r_y[:], r_y, r_y[:], r_v[:, 0:1], r_nw, r_nw[:], ALU.mult, ALU.mult, rd=[r_v])
        yield
        h.TT(r_y, r_y[:], r_y, r_y[:], r_nb, r_nb[:], ALU.add)
        yield
        h.STT(r_y, r_y[:], r_sV, r_sV[:], r_bs[:, 0:1], r_y, r_y[:], ALU.mult, ALU.add, rd=[r_bs])
        yield
        pg = PSr()
        yield
        h.MM(pg, pg[0:64, 0:64], r_sgT, r_sgT[:, cs], r_g2, r_g2[:, 0:64])
        yield
        h.MM(pg, pg[64:128, 0:64], r_sgT, r_sgT[:, cs], r_g2, r_g2[:, 64:128])
        yield
        for hh in range(2):
            ps_ = slice(hh * 64, (hh + 1) * 64)
            yield
            h.TT(r_Yb, r_Yb[ps_, ps_], r_y, r_y[ps_, :], pg, pg[ps_, 0:64], ALU.mult)
            yield
        p = PSr()
        yield
        h.MM(p, p[:, 0:64], r_Yb, r_Yb[:], cst, cst[:, C_I2, 0:64])
        yield
        h.CP(yst[3], yst[3][:, cs], p, p[:, 0:64])
        yield

    def tm_proj(ut, st_i, j):
        o, n = OFF["T0"]
        p = PF()
        for k in range(8):
            h.MM(p, p[:, 0:n], ut, ut[:, k, j * 128:(j + 1) * 128], W, W[:, k, o:o + n], st=(k == 0), sp=(k == 7))
        if "a" in en:
            h.CP(a_V, a_V[:, st_i * 4 + j, 0:128], p, p[:, 0:128], eng="act")
        if "b" in en:
            h.CP(g_v[j], g_v[j][:], p, p[:, 128:256], eng="act")
            h.ACT(g_sg[j], g_sg[j][:], p, p[:, 256:384], AF.Silu)
            h.CP(g_kraw[j], g_kraw[j][:], p, p[:, 384:448])
        if "c" in en:
            o, n = OFF["T1"]
            p = PF()
            for k in range(8):
                h.MM(p, p[:, 0:n], ut, ut[:, k, j * 128:(j + 1) * 128], W, W[:, k, o:o + n], st=(k == 0), sp=(k == 7))
            h.TT(s_dtr[j], s_dtr[j][:], s_dtb, s_dtb[:], p, p[:, 256:260], ALU.add)
            h.ACT(s_sz[j], s_sz[j][:], p, p[:, 0:256], AF.Silu)

    def gla_step(ut, j):
        js = slice(j * 128, (j + 1) * 128)
        yield
        tri = cst[:, C_TRIG, :]
        pz = PSg()
        yield
        h.MM(pz, pz[:, 0:64], g_glr, g_glr[:, js], gW2, gW2[:, :])
        yield
        h.ACT(g_e1, g_e1[:], pz, pz[:, 0:64], AF.Exp, scale=-1.0)
        yield
        h.ACT(g_l, g_l[:], g_e1, g_e1[:], AF.Ln, bias=1.0)
        yield
        pG = PSg()
        yield
        h.MM(pG, pG[:, 0:64], cst, tri, g_l, g_l[:])
        yield
        pGT = PSg()
        yield
        h.MM(pGT, pGT[0:64, :], g_l, g_l[:], cst, tri)
        yield
        h.ACT(g_eGT, g_eGT[:], pGT, pGT[0:64, :], AF.Exp)
        yield
        h.ACT(g_enGT, g_enGT[:], pGT, pGT[0:64, :], AF.Exp, scale=-1.0)
        yield
        h.ACT(g_enG, g_enG[:], pG, pG[:, 0:64], AF.Exp, scale=-1.0)
        yield
        h.CP(g_dec, g_dec[:], g_eGT, g_eGT[:, 127:128])
        yield
        h.STT(g_qin, g_qin[:], g_qT, g_qT[:, js], 0.125, g_eGT, g_eGT[:], ALU.mult, ALU.mult)
        yield
        h.TT(g_ktT, g_ktT[:], g_kT, g_kT[:, js], g_enGT, g_enGT[:], ALU.mult)
        yield
        h.TT(g_kt, g_kt[:], g_enG, g_enG[:], g_kraw[j], g_kraw[j][:], ALU.mult)
        yield
        pA = PSg()
        yield
        h.MM(pA, pA[:, :], g_ktT, g_ktT[:], g_qin, g_qin[:])
        yield
        h.TT(g_att, g_att[:], pA, pA[:, :], cst, cst[:, C_TRI, :], ALU.mult)
        yield
        pY = PSg()
        yield
        h.MM(pY, pY[:, :], g_att, g_att[:], g_v[j], g_v[j][:], st=True, sp=False)
        h.MM(pY, pY[:, :], g_qin, g_qin[:], g_Sb, g_Sb[:], st=False, sp=True)
        yield
        pK = PSg()
        yield
        h.MM(pK, pK[0:64, :], g_kt, g_kt[:], g_v[j], g_v[j][:])
        yield
        h.TT(g_S, g_S[:], g_S, g_S[:], pK, pK[0:64, :], ALU.add)
        yield
        h.TS(g_S, g_S[:], g_S, g_S[:], g_dec[:, 0:1], ALU.mult, rd=[g_dec])
        yield
        h.CP(g_Sb, g_Sb[:], g_S, g_S[:], eng="act")
        yield
        h.ACT(g_sq, g_sq[:], pY, pY[:, :], AF.Square, accum_out=g_ss[:], scale=(1.0 / 128.0) ** 0.5, wr=[g_ss])
        yield
        h.ACT(g_ss, g_ss[:], g_ss, g_ss[:], AF.Sqrt, bias=epsb[:], rd=[epsb])
        yield
        S.op("dve", "reciprocal", [g_ss], [g_ss], out=g_ss[:], in_=g_ss[:])
        yield
        h.STT(g_y, g_y[:], pY, pY[:, :], g_ss[:, 0:1], gnb, gnb[:], ALU.mult, ALU.mult, rd=[g_ss])
        yield
        h.TT(g_yb, g_yb[:], g_y, g_y[:], g_sg[j], g_sg[j][:], ALU.mult)
        yield
        put_out(g_yb, 1, j)
        yield


    def put_out(src, br, j):
        cnt["tro"] = cnt.get("tro", 0) + 1
        p = trslots[{0: 4 + cnt["tro"] % 2, 1: 0, 2: 3}[br]]
        h.TR(p, p[:, :], src, src[:], cstb, identb)
        h.CP(yst[br], yst[br][:, j * 128:(j + 1) * 128], p, p[:, :])

    for st_i in range(NST):
        ut = UTt[st_i % 2]
        t0 = st_i * ST
        for k in range(0, 8, 4):
            S.dma("sp", ut[:, k:k + 4, :], UTv[:, k:k + 4, t0:t0 + ST], writes=[ut], partial=(k > 0))
        if "b" in en:
            fm_proj("gq", ut, g_qT, g_qT[:])
            fm_proj("gk", ut, g_kT, g_kT[:])
            fm_proj("gglr", ut, g_glr, g_glr[0:16, :])
        if "c" in en:
            ssd_prep(ut)
        if "a" in en:
            att_prep(ut, t0)
        if "d" in en:
            rw_prep(ut)
        cur["tm_done"] = set()
        att_g = att_run(st_i) if "a" in en else None
        for j in range(4):
            tm_proj(ut, st_i, j)
            cur["tm_done"].add(j)
            threads = []
            if "d" in en:
                def _tr(j=j):
                    yield from rw_chunk(2 * j)
                    yield from rw_chunk(2 * j + 1)
                threads.append(_tr())
            if "b" in en:
                for _ in gla_step(ut, j):
                    pass
            if "c" in en:
                threads.append(ssd_step(ut, j))
            while threads:
                for g_ in list(threads):
                    try:
                        next(g_)
                    except StopIteration:
                        threads.remove(g_)
        while att_g is not None:
            try:
                next(att_g)
            except StopIteration:
                att_g = None
        for br, e_ in enumerate(("a", "b", "c", "d")):
            if e_ in en:
                S.dma("sp", yT[br * 128:(br + 1) * 128, t0:t0 + ST], yst[br][:], reads=[yst[br]], writes=[yT], partial=True)
    S.finish_wait("sp")
    S.emit()
    return nc


import numpy as np
def bc(v, n=128):
    return np.ascontiguousarray(np.broadcast_to(np.asarray(v, np.float32)[None, :], (n, len(v))))
def mix_inputs(P, g, SL, lam_init):
    cols, off = core_cols(g)
    m = {"W": np.ascontiguousarray(P["w_in"][:, cols]), "consts": make_consts()}
    hs = slice(g * 64, (g + 1) * 64)
    m["gla_w2aug"] = np.ascontiguousarray(np.concatenate([P["gla_gate_w2"][:, hs], P["gla_gate_b"][None, hs]], 0))
    m["gla_norm_bc"] = bc(P["gla_norm"])

    gr = g // 2
    heads = [2 * g, 2 * g + 1, 2 * (g ^ 1), 2 * (g ^ 1) + 1]
    xcols = np.concatenate([g * 128 + np.arange(128), (g ^ 1) * 128 + np.arange(128)])
    ch = [xcols[:128], xcols[128:], 512 + gr * 128 + np.arange(128), 768 + gr * 128 + np.arange(128)]
    cw = np.zeros((128, 16), np.float32); cb = np.zeros((128, 4), np.float32)
    for gi, c in enumerate(ch):
        cw[:, gi * 4:(gi + 1) * 4] = P["ssd_conv_w"][:, c].T
        cb[:, gi] = P["ssd_conv_b"][c]
    m["ssd_convw"] = cw; m["ssd_convb"] = cb
    m["ssd_dtb_bc"] = bc(P["ssd_dt_bias"][heads]); m["ssd_alog_bc"] = bc(P["ssd_a_log"][heads]); m["ssd_d_bc"] = bc(P["ssd_d"][heads])
    m["ssd_norm_bc"] = bc(P["ssd_norm"][xcols])
    half = 32
    inv_freq = (10000.0 ** (-np.arange(half, dtype=np.float32) / half)).astype(np.float32)
    ang = np.arange(SL, dtype=np.float32)[None, :] * inv_freq[:, None]
    cos, sin = np.cos(ang).astype(np.float32), np.sin(ang).astype(np.float32)
    m["ropeC"] = np.ascontiguousarray(np.concatenate([cos, cos, cos, cos], 0))
    m["ropeS"] = np.ascontiguousarray(np.concatenate([-sin, sin, -sin, sin], 0))
    for i, k in enumerate(("da_lambda_q1", "da_lambda_k1", "da_lambda_q2", "da_lambda_k2")):
        m["da_l%d" % i] = bc(P[k])
    m["da_norm_bc"] = bc(P["da_norm"])
    m["lamc"] = bc(np.array([1.0 - lam_init, -lam_init], np.float32))
    RWS = [512, 512, 512, 64, 64, 128]
    offs = np.cumsum([0] + RWS)
    my = slice(g * 128, (g + 1) * 128)
    mu = np.zeros((128, 6), np.float32)
    mu[:, 0] = P["rw_mu"][offs[0]:offs[1]][my]; mu[:, 1] = P["rw_mu"][offs[1]:offs[2]][my]; mu[:, 2] = P["rw_mu"][offs[2]:offs[3]][my]
    mu[:64, 3] = P["rw_mu"][offs[3]:offs[4]]; mu[:64, 4] = P["rw_mu"][offs[4]:offs[5]]; mu[:, 5] = P["rw_mu"][offs[5]:offs[6]]
    m["rw_mu"] = mu
    m["rw_par"] = np.ascontiguousarray(np.stack([P["rw_w0"][my], P["rw_a0"][my], P["rw_k_k"][my], P["rw_k_a"][my], P["rw_r_k"][my]], 1))
    m["rw_nw_st"] = np.ascontiguousarray(P["rw_norm_w"][my].reshape(2, 1, 64).repeat(64, 1).reshape(128, 64))
    m["rw_nb_st"] = np.ascontiguousarray(P["rw_norm_b"][my].reshape(2, 1, 64).repeat(64, 1).reshape(128, 64))
    m["rw_w2"] = np.ascontiguousarray(P["rw_w2"][:, my]); m["rw_a2"] = np.ascontiguousarray(P["rw_a2"][:, my]); m["rw_g2"] = np.ascontiguousarray(P["rw_g2"][:, my])
    return m


SEQ = 8192
NCORE = 8


def _g8(v):
    return np.ascontiguousarray(np.asarray(v, np.float32).reshape(8, 128).T)


def _ffn_inputs(pfx, inp, which, l):
    return {pfx + "_g": _g8(inp[which + "_norm"][l]), pfx + "_wg": np.ascontiguousarray(inp[which + "_wg"][l]),
            pfx + "_wu": np.ascontiguousarray(inp[which + "_wu"][l]), pfx + "_wd": np.ascontiguousarray(inp[which + "_wd"][l])}


def kernel(**inp):
    inp = {k: np.asarray(v) for k, v in inp.items()}
    x = inp["x"].astype(np.float32)
    L = inp["w_in"].shape[0]
    cores = list(range(NCORE))
    hT = [np.ascontiguousarray(x[c // 4, (c % 4) * 2048:(c % 4 + 1) * 2048].T) for c in cores]
    P0 = build_tok(False, True, False)
    Pmid = build_tok(True, True, False)
    Plast = build_tok(True, False, True)
    Pmix = build_mix(SEQ, 0.0)
    maps = []
    for c in cores:
        m = {"hT_in": hT[c], "mixn_g": _g8(inp["mix_norm"][0])}
        m.update(_ffn_inputs("f1", inp, "ffn1", 0))
        maps.append(m)
    res = run_bass_kernel_spmd(P0, maps, core_ids=cores).results
    hT = [np.asarray(r["hT_out"]) for r in res]
    uT = [np.asarray(r["uT_out"]) for r in res]
    for l in range(L):
        lam_init = 0.8 - 0.6 * math.exp(-0.3 * l)
        Pl = {k: inp[k][l] for k in inp if k not in ("x", "final_norm")}
        UTb = [np.ascontiguousarray(np.concatenate([uT[b * 4 + j] for j in range(4)], axis=1)) for b in range(2)]
        maps = []
        for c in cores:
            m = mix_inputs(Pl, c % 4, SEQ, lam_init)
            m["UT"] = UTb[c // 4]
            maps.append(m)
        res = run_bass_kernel_spmd(Pmix, maps, core_ids=cores).results
        yT = [np.asarray(r["yT"]) for r in res]
        yfull = []
        for b in range(2):
            rows = []
            for br in range(4):
                for g in range(4):
                    rows.append(yT[b * 4 + g][br * 128:(br + 1) * 128])
            yfull.append(np.concatenate(rows, axis=0))
        gate_cols = np.ascontiguousarray(inp["w_in"][l][:, 6424:6424 + 4096])
        gb = np.ascontiguousarray(inp["gate_b"][l].reshape(4, 8, 128).transpose(2, 0, 1).reshape(128, 32))
        maps = []
        for c in cores:
            b, j = c // 4, c % 4
            m = {"hT_in": hT[c], "uT_in": uT[c], "yT_in": np.ascontiguousarray(yfull[b][:, j * 2048:(j + 1) * 2048]),
                 "w_gate": gate_cols, "w_branch": np.ascontiguousarray(inp["w_branch"][l].reshape(4 * 512, 1024)),
                 "w_out": np.ascontiguousarray(inp["w_out"][l]), "gate_b": gb}
            m.update(_ffn_inputs("f2", inp, "ffn2", l))
            if l + 1 < L:
                m.update(_ffn_inputs("f1", inp, "ffn1", l + 1))
                m["mixn_g"] = _g8(inp["mix_norm"][l + 1])
            else:
                m["final_g"] = _g8(inp["final_norm"])
            maps.append(m)
        res = run_bass_kernel_spmd(Pmid if l + 1 < L else Plast, maps, core_ids=cores).results
        hT = [np.asarray(r["hT_out"]) for r in res]
        if l + 1 < L:
            uT = [np.asarray(r["uT_out"]) for r in res]
    out = np.zeros((2, SEQ, 1024), np.float32)
    for c in cores:
        out[c // 4, (c % 4) * 2048:(c % 4 + 1) * 2048] = hT[c].T
    return out
```
